# Optimizing a Trainium2 kernel written in Bass

```python
import math
import jax, jax.numpy as jnp
from jax import lax
import numpy as np

D_MODEL = 1024
BATCH = 2
SEQ = 8192
DEPTH = 4

N_EVEN = (DEPTH + 1) // 2
N_ODD = DEPTH // 2
ALPHA = (2 * DEPTH) ** 0.25
BETA = (8 * DEPTH) ** -0.25
LN_EPS = 1e-5
SC_WIDTH = D_MODEL
SC_KERNEL = 3
SSM_HEAD_DIM = 64
SSM_INNER = D_MODEL
SSM_HEADS = SSM_INNER // SSM_HEAD_DIM
SSM_GROUPS = 2
SSM_STATE = 128
SSM_CONV = 4
SSM_CHUNK = 128
SSM_CONV_DIM = SSM_INNER + 2 * SSM_GROUPS * SSM_STATE
IN_COLS = 3 * SC_WIDTH + SSM_INNER + SSM_CONV_DIM + SSM_HEADS
MIX_WIDTH = SC_WIDTH + SSM_INNER
CONF_KERNEL = 31
N_EXPERTS = 32
TOP_K = 4
D_EXPERT = D_MODEL
SWIGLU_LIMIT = 7.0
SWIGLU_ALPHA = 1.702
MOE_BLOCK = 512

kernel_name = "hybrid_shortconv_ssd_conformer_moe_deepnorm"


def layer_norm(x, g, b):
    xf = x.astype(jnp.float32)
    mu = jnp.mean(xf, axis=-1, keepdims=True)
    var = jnp.mean(jnp.square(xf - mu), axis=-1, keepdims=True)
    y = (xf - mu) * lax.rsqrt(var + LN_EPS) * g.astype(jnp.float32) + b.astype(jnp.float32)
    return y.astype(x.dtype)


def causal_dwconv(u, w):
    k, c = w.shape
    return lax.conv_general_dilated(
        u, w.astype(u.dtype)[:, None, :], window_strides=(1,),
        padding=[(k - 1, 0)], dimension_numbers=("NWC", "WIO", "NWC"),
        feature_group_count=c)


def ssd_chunked(x, dt, a, bm, cm):
    b, s, h, p = x.shape
    g, n = bm.shape[2], bm.shape[3]
    r = h // g
    c, l = s // SSM_CHUNK, SSM_CHUNK
    xd = (x * dt[..., None]).reshape(b, c, l, g, r, p)
    da = jnp.moveaxis((dt * a).reshape(b, c, l, g, r), 2, -1)
    a_cum = jnp.cumsum(da, axis=-1)
    bc = bm.reshape(b, c, l, g, n)
    cc = cm.reshape(b, c, l, g, n)
    seg = a_cum[..., :, None] - a_cum[..., None, :]
    causal = jnp.tril(jnp.ones((l, l), dtype=bool))
    decay_ls = jnp.exp(jnp.where(causal, seg, -jnp.inf))
    cb = jnp.einsum("bclgn,bcsgn->bcgls", cc, bc)
    y_diag = jnp.einsum("bcgls,bcgrls,bcsgrp->bclgrp", cb, decay_ls, xd)
    decay_to_end = jnp.exp(a_cum[..., -1:] - a_cum)
    states = jnp.einsum("bclgn,bcgrl,bclgrp->bcgrpn", bc, decay_to_end, xd)
    chunk_decay = jnp.exp(a_cum[..., -1])

    def step(carry, inp):
        st, dec = inp
        return carry * dec[..., None, None] + st, carry

    init = jnp.zeros((b, g, r, p, n), jnp.float32)
    _, prev = lax.scan(step, init, (jnp.moveaxis(states, 1, 0), jnp.moveaxis(chunk_decay, 1, 0)))
    prev = jnp.moveaxis(prev, 0, 1)
    y_off = jnp.einsum("bclgn,bcgrpn,bcgrl->bclgrp", cc, prev, jnp.exp(a_cum))
    return (y_diag + y_off).reshape(b, s, h, p)


def shortconv_ssd_mix(h, w_in, sc_conv_w, ssm_conv_w, ssm_conv_b, dt_bias, a_log, d_skip, norm_w, w_out):
    b, s, _ = h.shape
    proj = h @ w_in
    cuts = [SC_WIDTH, 2 * SC_WIDTH, 3 * SC_WIDTH, 3 * SC_WIDTH + SSM_INNER,
            3 * SC_WIDTH + SSM_INNER + SSM_CONV_DIM]
    sc_x, sc_pre, sc_post, z, xbc, dt_raw = jnp.split(proj, cuts, axis=-1)
    y_sc = sc_post * causal_dwconv(sc_pre * sc_x, sc_conv_w)
    xbc = jax.nn.silu(causal_dwconv(xbc, ssm_conv_w) + ssm_conv_b)
    gn = SSM_GROUPS * SSM_STATE
    xs, bm, cm = jnp.split(xbc, [SSM_INNER, SSM_INNER + gn], axis=-1)
    f32 = jnp.float32
    dt = jax.nn.softplus(dt_raw.astype(f32) + dt_bias.astype(f32))
    a = -jnp.exp(a_log.astype(f32))
    xh = xs.reshape(b, s, SSM_HEADS, SSM_HEAD_DIM).astype(f32)
    y = ssd_chunked(xh, dt, a,
                    bm.reshape(b, s, SSM_GROUPS, SSM_STATE).astype(f32),
                    cm.reshape(b, s, SSM_GROUPS, SSM_STATE).astype(f32))
    y = y + xh * d_skip.astype(f32)[:, None]
    y = y.reshape(b, s, SSM_INNER) * jax.nn.silu(z.astype(f32))
    yg = y.reshape(b, s, SSM_GROUPS, SSM_INNER // SSM_GROUPS)
    yg = yg * lax.rsqrt(jnp.mean(jnp.square(yg), axis=-1, keepdims=True) + LN_EPS)
    y_ssm = (yg.reshape(b, s, SSM_INNER) * norm_w.astype(f32)).astype(h.dtype)
    return jnp.concatenate([y_sc, y_ssm], axis=-1) @ w_out


def conformer_conv(h, w_pw1, b_pw1, w_dw, b_dw, ln_g, ln_b, w_pw2, b_pw2):
    u = h @ w_pw1 + b_pw1
    u = u[..., :D_MODEL] * jax.nn.sigmoid(u[..., D_MODEL:])
    u = causal_dwconv(u, w_dw) + b_dw
    u = jax.nn.silu(layer_norm(u, ln_g, ln_b))
    return u @ w_pw2 + b_pw2


def moe_ffn(h, w_r, b_r, w_gu, b_gu, w_down, b_down):
    bsz, s, d = h.shape
    t = bsz * s
    n_assign = t * TOP_K
    xt = h.reshape(t, d)
    logits = (xt @ w_r + b_r).astype(jnp.float32)
    top_vals, top_idx = lax.top_k(logits, TOP_K)
    gates = jax.nn.softmax(top_vals, axis=-1)
    flat_e = top_idx.reshape(-1)
    order = jnp.argsort(flat_e)
    sorted_e = flat_e[order]
    tok = order // TOP_K
    counts = jnp.bincount(flat_e, length=N_EXPERTS)
    starts = jnp.cumsum(counts) - counts
    padded = ((counts + MOE_BLOCK - 1) // MOE_BLOCK) * MOE_BLOCK
    pad_ends = jnp.cumsum(padded)
    pad_starts = pad_ends - padded
    dest = pad_starts[sorted_e] + (jnp.arange(n_assign) - starts[sorted_e])
    n_blocks = -(-n_assign // MOE_BLOCK) + N_EXPERTS
    buf = jnp.zeros((n_blocks * MOE_BLOCK, d), h.dtype).at[dest].set(xt[tok])
    block_e = jnp.minimum(
        jnp.searchsorted(pad_ends, jnp.arange(n_blocks) * MOE_BLOCK, side="right"),
        N_EXPERTS - 1)

    def expert_block(args):
        xb, e = args
        gu = xb @ w_gu[e] + b_gu[e]
        gate = jnp.minimum(gu[:, :D_EXPERT], SWIGLU_LIMIT)
        up = jnp.clip(gu[:, D_EXPERT:], -SWIGLU_LIMIT, SWIGLU_LIMIT)
        act = (up + 1.0) * (gate * jax.nn.sigmoid(SWIGLU_ALPHA * gate))
        return act @ w_down[e] + b_down[e]

    yb = lax.map(expert_block, (buf.reshape(n_blocks, MOE_BLOCK, d), block_e)).reshape(-1, d)
    y_assign = yb[dest] * gates.reshape(-1)[order].astype(h.dtype)[:, None]
    y = jax.ops.segment_sum(y_assign, tok, num_segments=t)
    return y.reshape(bsz, s, d)


def setup_inputs(seed: int = 0) -> dict:
    key = jax.random.key(seed)
    ks = jax.random.split(key, 32)
    f32 = jnp.float32
    nrm = lambda k, shape, scale: jax.random.normal(k, shape, f32) * scale
    dt0 = jnp.exp(jax.random.uniform(ks[6], (N_EVEN, SSM_HEADS), f32)
                  * (math.log(0.1) - math.log(0.001)) + math.log(0.001))
    return {
        "x": nrm(ks[0], (BATCH, SEQ, D_MODEL), 1.0),
        "mix_w_in": nrm(ks[1], (N_EVEN, D_MODEL, IN_COLS), D_MODEL ** -0.5),
        "sc_conv_w": nrm(ks[2], (N_EVEN, SC_KERNEL, SC_WIDTH), SC_KERNEL ** -0.5),
        "ssm_conv_w": nrm(ks[3], (N_EVEN, SSM_CONV, SSM_CONV_DIM), SSM_CONV ** -0.5),
        "ssm_conv_b": nrm(ks[4], (N_EVEN, SSM_CONV_DIM), 0.02),
        "ssm_dt_bias": dt0 + jnp.log(-jnp.expm1(-dt0)),
        "ssm_a_log": jnp.log(jax.random.uniform(ks[7], (N_EVEN, SSM_HEADS), f32, 1.0, 16.0)),
        "ssm_d": 1.0 + nrm(ks[8], (N_EVEN, SSM_HEADS), 0.1),
        "ssm_norm_w": 1.0 + nrm(ks[9], (N_EVEN, SSM_INNER), 0.1),
        "mix_w_out": nrm(ks[10], (N_EVEN, MIX_WIDTH, D_MODEL), BETA * MIX_WIDTH ** -0.5),
        "conf_w_pw1": nrm(ks[11], (N_ODD, D_MODEL, 2 * D_MODEL), D_MODEL ** -0.5),
        "conf_b_pw1": nrm(ks[12], (N_ODD, 2 * D_MODEL), 0.02),
        "conf_w_dw": nrm(ks[13], (N_ODD, CONF_KERNEL, D_MODEL), CONF_KERNEL ** -0.5),
        "conf_b_dw": nrm(ks[14], (N_ODD, D_MODEL), 0.02),
        "conf_ln_g": 1.0 + nrm(ks[15], (N_ODD, D_MODEL), 0.1),
        "conf_ln_b": nrm(ks[16], (N_ODD, D_MODEL), 0.02),
        "conf_w_pw2": nrm(ks[17], (N_ODD, D_MODEL, D_MODEL), BETA * D_MODEL ** -0.5),
        "conf_b_pw2": nrm(ks[18], (N_ODD, D_MODEL), 0.02),
        "router_w": nrm(ks[19], (DEPTH, D_MODEL, N_EXPERTS), D_MODEL ** -0.5),
        "router_b": nrm(ks[20], (DEPTH, N_EXPERTS), 0.01),
        "exp_w_gu": nrm(ks[21], (DEPTH, N_EXPERTS, D_MODEL, 2 * D_EXPERT), D_MODEL ** -0.5),
        "exp_b_gu": nrm(ks[22], (DEPTH, N_EXPERTS, 2 * D_EXPERT), 0.02),
        "exp_w_down": nrm(ks[23], (DEPTH, N_EXPERTS, D_EXPERT, D_MODEL), BETA * D_EXPERT ** -0.5),
        "exp_b_down": nrm(ks[24], (DEPTH, N_EXPERTS, D_MODEL), 0.02),
        "ln_mix_g": 1.0 + nrm(ks[25], (DEPTH, D_MODEL), 0.1),
        "ln_mix_b": nrm(ks[26], (DEPTH, D_MODEL), 0.02),
        "ln_ffn_g": 1.0 + nrm(ks[27], (DEPTH, D_MODEL), 0.1),
        "ln_ffn_b": nrm(ks[28], (DEPTH, D_MODEL), 0.02),
    }


def reference(x, mix_w_in, sc_conv_w, ssm_conv_w, ssm_conv_b, ssm_dt_bias, ssm_a_log, ssm_d,
              ssm_norm_w, mix_w_out, conf_w_pw1, conf_b_pw1, conf_w_dw, conf_b_dw, conf_ln_g,
              conf_ln_b, conf_w_pw2, conf_b_pw2, router_w, router_b, exp_w_gu, exp_b_gu,
              exp_w_down, exp_b_down, ln_mix_g, ln_mix_b, ln_ffn_g, ln_ffn_b):
    h = x
    for i in range(DEPTH):
        j = i // 2
        if i % 2 == 0:
            mix = shortconv_ssd_mix(h, mix_w_in[j], sc_conv_w[j], ssm_conv_w[j], ssm_conv_b[j],
                                    ssm_dt_bias[j], ssm_a_log[j], ssm_d[j], ssm_norm_w[j],
                                    mix_w_out[j])
        else:
            mix = conformer_conv(h, conf_w_pw1[j], conf_b_pw1[j], conf_w_dw[j], conf_b_dw[j],
                                 conf_ln_g[j], conf_ln_b[j], conf_w_pw2[j], conf_b_pw2[j])
        h = layer_norm(ALPHA * h + mix, ln_mix_g[i], ln_mix_b[i])
        ffn = moe_ffn(h, router_w[i], router_b[i], exp_w_gu[i], exp_b_gu[i],
                      exp_w_down[i], exp_b_down[i])
        h = layer_norm(ALPHA * h + ffn, ln_ffn_g[i], ln_ffn_b[i])
    return h
```

```python
from contextlib import ExitStack
import numpy as np
import concourse.bass as bass
import concourse.mybir as mybir
from concourse.bass_utils import run_bass_kernel_spmd

F32 = mybir.dt.float32
BF16 = mybir.dt.bfloat16
ALU = mybir.AluOpType
ACTF = mybir.ActivationFunctionType
AX = mybir.AxisListType

D = 1024
NTOK = 2048
NCH = NTOK // 128
NE = 32
CAP = 384
NJC = CAP // 128
DEPTH = 4
ALPHA = (2 * DEPTH) ** 0.25
LN_EPS = 1e-5
SW_LIMIT = 7.0
SW_ALPHA = 1.702

EP = 30000
NS = 16
ENGS = ("pe", "dve", "act", "pool", "sp")


class T:
    __slots__ = ("name", "w", "r")

    def __init__(self, name=""):
        self.name = name
        self.w = None
        self.r = {}


class Prog:
    def __init__(self, nc):
        self.nc = nc
        self.es = ExitStack()
        self.phs = []
        self.ops = {e: [] for e in ENGS}
        self.ccount = {e: 0 for e in ENGS}
        self.dcount = {e: 0 for e in ENGS}
        self.known = {e: {} for e in ENGS}
        self.sems = {}
        self.nsem = 0
        self.nname = 0
        self.xcount = 0

    def _nm(self, name):
        self.nname += 1
        return "%s_%d" % (name, self.nname)

    def sbuf(self, name, shape, dt, persistent=False):
        st = self.es if (persistent or not self.phs) else self.phs[-1]
        return st.enter_context(self.nc.sbuf_tensor(self._nm(name), list(shape), dt))

    def psum(self, name, shape, dt, persistent=False):
        st = self.es if (persistent or not self.phs) else self.phs[-1]
        return st.enter_context(self.nc.psum_tensor(self._nm(name), list(shape), dt))

    def begin_phase(self):
        self.phs.append(ExitStack())

    def end_phase(self):
        self.barrier()
        self.flush()
        self.phs.pop().close()

    def _sem(self, key):
        s = self.sems.get(key)
        if s is None:
            self.nsem += 1
            s = self.es.enter_context(self.nc.semaphore("s%d" % self.nsem))
            self.sems[key] = s
        return s

    def _need(self, eng, tok, waits):
        if tok is None:
            return
        key, val = tok
        if key[0] == "c" and key[1] == eng and eng == "pe":
            return
        if self.known[eng].get(key, 0) >= val:
            return
        self.known[eng][key] = val
        waits.append(tok)

    def _deps(self, eng, reads, writes):
        waits = []
        for t in reads:
            self._need(eng, t.w, waits)
        for t in writes:
            self._need(eng, t.w, waits)
            for k, v in t.r.items():
                self._need(eng, (k, v), waits)
        return waits

    def _mark(self, tok, reads, writes):
        key, val = tok
        ws = set(id(t) for t in writes)
        for t in writes:
            t.w = tok
            t.r = {}
        for t in reads:
            if id(t) in ws:
                continue
            if t.r.get(key, 0) < val:
                t.r[key] = val

    def op(self, eng, fn, reads=(), writes=()):
        waits = self._deps(eng, reads, writes)
        self.ccount[eng] += 1
        tok = (("c", eng), self.ccount[eng])
        self.ops[eng].append((waits, fn, tok))
        self._mark(tok, reads, writes)
        return tok

    def dma(self, eng, fn, reads=(), writes=()):
        waits = self._deps(eng, reads, writes)
        n = self.dcount[eng]
        self.dcount[eng] += 1
        slot = n % NS
        key = ("d", eng, slot)
        if n >= NS:
            self._need(eng, (key, 16 * (n // NS)), waits)
        tok = (key, 16 * (n // NS + 1))
        self.ops[eng].append((waits, fn, tok))
        self._mark(tok, reads, writes)
        return tok

    def coll(self, fn, reads=(), writes=()):
        waits = self._deps("pool", reads, writes)
        self.xcount += 1
        tok = (("x",), self.xcount)
        self.ops["pool"].append((waits, fn, tok))
        self._mark(tok, reads, writes)
        return tok

    def _all_tokens(self):
        toks = [(("x",), self.xcount)] if self.xcount else []
        for e in ENGS:
            if self.ccount[e]:
                toks.append((("c", e), self.ccount[e]))
            n = self.dcount[e]
            for slot in range(min(n, NS)):
                last = ((n - 1 - slot) // NS) * NS + slot
                toks.append((("d", e, slot), 16 * (last // NS + 1)))
        return toks

    def barrier(self, engs=ENGS):
        toks = self._all_tokens()
        for e in engs:
            waits = []
            for tok in toks:
                self._need(e, tok, waits)
            if waits:
                self.ops[e].append((waits, None, None))

    def _semval(self, tok):
        key, val = tok
        if key[0] == "c":
            ep = (val - 1) // EP
            return self._sem(("c", key[1], ep)), (val - 1) % EP + 1
        return self._sem(key), val

    def flush(self):
        nc = self.nc
        engobj = {"pe": "tensor", "dve": "vector", "act": "scalar", "pool": "gpsimd", "sp": "sync"}
        for e in ENGS:
            for waits, fn, tok in self.ops[e]:
                for w in waits:
                    self._semval(w)
                if tok is not None:
                    self._semval(tok)
        if not any(self.ops[e] for e in ENGS):
            return
        with nc.Block() as block:
            for e in ENGS:
                if not self.ops[e]:
                    continue

                def body(engine, e=e):
                    for waits, fn, tok in self.ops[e]:
                        for w in waits:
                            s, v = self._semval(w)
                            engine.wait_ge(s, v)
                        if fn is None:
                            continue
                        ins = fn(engine)
                        s, v = self._semval(tok)
                        ins.then_inc(s, 16 if tok[0][0] == "d" else 1)

                getattr(block, engobj[e])(body)
        self.ops = {e: [] for e in ENGS}

    def finish(self):
        self.barrier()
        self.flush()
        self.es.close()


class Consts:
    def __init__(self, p):
        self.t = T("consts")
        self.ident_f = p.sbuf("ident_f", [128, 128], F32, persistent=True)
        self.ident_b = p.sbuf("ident_b", [128, 128], BF16, persistent=True)
        self.lstrict_b = p.sbuf("lstrict_b", [128, 128], BF16, persistent=True)
        self.ones_b = p.sbuf("ones_b", [128, 128], BF16, persistent=True)
        self.iota_c = p.sbuf("iota_c", [128, CAP], F32, persistent=True)
        c = self
        t = self.t
        p.op("pool", lambda e: e.memset(c.ident_f[:], 0.0), writes=[t])
        p.op("pool", lambda e: e.affine_select(c.ident_f[:], c.ident_f[:], [[-1, 128]], ALU.not_equal, 1.0,
                                               base=0, channel_multiplier=1), reads=[t], writes=[t])
        p.op("pool", lambda e: e.tensor_copy(c.ident_b[:], c.ident_f[:]), reads=[t], writes=[t])
        p.op("pool", lambda e: e.memset(c.ones_b[:], 1.0), writes=[t])
        p.op("pool", lambda e: e.memset(c.lstrict_b[:], 1.0), writes=[t])
        p.op("pool", lambda e: e.affine_select(c.lstrict_b[:], c.lstrict_b[:], [[1, 128]], ALU.is_gt, 0.0,
                                               base=0, channel_multiplier=-1), reads=[t], writes=[t])
        p.op("pool", lambda e: e.iota(c.iota_c[:], [[1, CAP]], base=0, channel_multiplier=0,
                                      allow_small_or_imprecise_dtypes=True), writes=[t])


def bcast_rows(ap_1d, nparts=128):
    return ap_1d.partition_broadcast(nparts)


def emit_ln(p, x_ap, x_t, g_tile, b_tile, gb_t, st, st_t, out_ap=None, out_t=None, eng2="pool"):
    if out_ap is None:
        out_ap, out_t = x_ap, x_t
    stats = st[:, 0:12]
    mv = st[:, 12:14]
    rstd = st[:, 14:15]
    p.op("dve", lambda e: e.bn_stats(st[:, 0:6], x_ap[:, 0:512]), reads=[x_t], writes=[st_t])
    p.op("dve", lambda e: e.bn_stats(st[:, 6:12], x_ap[:, 512:1024]), reads=[x_t, st_t], writes=[st_t])
    p.op("dve", lambda e: e.bn_aggr(mv, stats), reads=[st_t], writes=[st_t])
    p.op("act", lambda e: e.activation(rstd, st[:, 13:14], ACTF.Sqrt, bias=LN_EPS), reads=[st_t], writes=[st_t])
    p.op("dve", lambda e: e.reciprocal(rstd, rstd), reads=[st_t], writes=[st_t])
    p.op("dve", lambda e: e.tensor_scalar(x_ap, x_ap, st[:, 12:13], rstd, ALU.subtract, ALU.mult),
         reads=[x_t, st_t], writes=[x_t])
    p.op(eng2, lambda e: e.tensor_tensor(x_ap, x_ap, g_tile[:], ALU.mult), reads=[x_t, gb_t], writes=[x_t])
    p.op(eng2, lambda e: e.tensor_tensor(out_ap, x_ap, b_tile[:], ALU.add), reads=[x_t, gb_t], writes=[out_t] if out_t is not x_t else [x_t])


def emit_moe(p, C, h1_d, h1_dt, h2_d, h2_dt, wr_d, br_d, wgu_of, bgu_d, wd_of, bd_d, lng_d, lnb_d, order=None):
    nc = p.nc
    p.begin_phase()
    acc = p.sbuf("acc", [128, NCH, D], F32)
    acc_t = [T("acc%d" % i) for i in range(NCH)]
    xtok = p.sbuf("xtok", [128, NCH, D], BF16)
    xtok_t = [T() for _ in range(NCH)]
    gates = p.sbuf("gates", [128, NCH, NE], F32)
    ghl = p.sbuf("ghl", [128, NCH, NE, 2], BF16)
    posm = p.sbuf("posm", [128, NCH, NE], F32)
    maskb = p.sbuf("maskb", [128, NCH, NE], BF16)
    rt_t = [T() for _ in range(NCH)]
    bguT = p.sbuf("bguT", [128, 16, NE], F32); bguT_t = T()
    PS = [p.psum("ps%d" % i, [128, 512], F32) for i in range(8)]
    PS_t = [T("ps%d" % i) for i in range(8)]
    p.begin_phase()
    wr = p.sbuf("wr", [128, 8, NE], F32); wr_t = T()
    brb = p.sbuf("brb", [128, NE], F32)
    bd = p.sbuf("bd", [NE, D], F32)
    bgu_raw = p.sbuf("bgu_raw", [NE, 2 * D], F32)

    p.dma("sp", lambda e: e.dma_start(out=wr[:], in_=wr_d.rearrange("(k p) n -> p k n", p=128)), writes=[wr_t])
    p.dma("sp", lambda e: e.dma_start(out=brb[:], in_=bcast_rows(br_d)), writes=[wr_t])
    p.dma("sp", lambda e: e.dma_start(out=bd[:], in_=bd_d), writes=[wr_t])
    p.dma("sp", lambda e: e.dma_start(out=bgu_raw[:], in_=bgu_d), writes=[wr_t])
    for c4 in range(4):
        ps = PS[7]
        for q in range(4):
            c = c4 * 4 + q
            p.op("pe", lambda e, c=c, q=q, ps=ps: e.transpose(ps[:, q * NE:(q + 1) * NE],
                                                             bgu_raw[:, c * 128:(c + 1) * 128], C.ident_f[0:NE, 0:NE]),
                 reads=[wr_t, C.t], writes=[PS_t[7]])
        p.op("dve", lambda e, c4=c4, ps=ps: e.tensor_copy(bguT[:, c4 * 4:(c4 + 1) * 4, :],
                                                          ps[:, 0:4 * NE].rearrange("p (q e) -> p q e", q=4)),
             reads=[PS_t[7]], writes=[bguT_t])

    xin = [p.sbuf("xin%d" % i, [128, D], F32) for i in range(2)]
    xin_t = [T() for _ in range(2)]
    xT = [p.sbuf("xT%d" % i, [128, 8, 128], F32) for i in range(2)]
    xT_t = [T() for _ in range(2)]
    rs = [p.sbuf("rs%d" % i, [128, 96], F32) for i in range(2)]
    rs_t = [T() for _ in range(2)]
    gT = [p.sbuf("gT%d" % i, [NE, 128], F32) for i in range(2)]
    gT_t = [T() for _ in range(2)]
    for i in range(NCH):
        b = i % 2
        x, x_t = xin[b], xin_t[b]
        p.dma("sp", lambda e, x=x, i=i: e.dma_start(out=x[:], in_=h1_d[i * 128:(i + 1) * 128, :]),
              reads=[h1_dt], writes=[x_t])
        p.op("act", lambda e, x=x, i=i: e.copy(xtok[:, i, :], x[:]), reads=[x_t], writes=[xtok_t[i]])
        for half in range(2):
            ps, ps_t = PS[half], PS_t[half]
            for q in range(4):
                dc = half * 4 + q
                p.op("pe", lambda e, x=x, dc=dc, q=q, ps=ps: e.transpose(ps[:, q * 128:(q + 1) * 128],
                                                                     x[:, dc * 128:(dc + 1) * 128], C.ident_f[:]),
                     reads=[x_t, C.t], writes=[ps_t])
            p.op("dve" if half == 0 else "act",
                 (lambda e, ps=ps, b=b, half=half: e.tensor_copy(
                     xT[b][:, half * 4:(half + 1) * 4, :], ps[:].rearrange("p (q t) -> p q t", q=4))) if half == 0 else
                 (lambda e, ps=ps, b=b, half=half: e.copy(
                     xT[b][:, half * 4:(half + 1) * 4, :], ps[:].rearrange("p (q t) -> p q t", q=4))),
                 reads=[ps_t], writes=[xT_t[b]])
        for kc in range(8):
            p.op("pe", lambda e, b=b, kc=kc: e.matmul(PS[2][:, 0:NE], xT[b][:, kc, :], wr[:, kc, :],
                                                      start=(kc == 0), stop=(kc == 7)),
                 reads=[xT_t[b], wr_t], writes=[PS_t[2]])
        r, r_t = rs[b], rs_t[b]
        lg = r[:, 0:32]
        top8 = r[:, 32:40]
        nm1 = r[:, 40:41]
        ssum = r[:, 41:42]
        ex = r[:, 48:80]
        p.op("dve", lambda e, lg=lg: e.tensor_tensor(lg, PS[2][:, 0:NE], brb[:], ALU.add),
             reads=[PS_t[2], wr_t], writes=[r_t])
        p.op("dve", lambda e, lg=lg, top8=top8: e.max(top8, lg), reads=[r_t], writes=[r_t])
        p.op("dve", lambda e, r=r: e.tensor_scalar(r[:, 40:41], r[:, 32:33], -1.0, None, ALU.mult),
             reads=[r_t], writes=[r_t])
        p.op("act", lambda e, r=r: e.activation(r[:, 48:80], r[:, 0:32], ACTF.Exp, bias=r[:, 40:41]),
             reads=[r_t], writes=[r_t])
        p.op("dve", lambda e, r=r, i=i: e.tensor_scalar(gates[:, i, :], r[:, 0:32], r[:, 35:36], None, ALU.is_ge),
             reads=[r_t], writes=[rt_t[i]])
        p.op("dve", lambda e, i=i: e.tensor_copy(maskb[:, i, :], gates[:, i, :]), reads=[rt_t[i]], writes=[rt_t[i]])
        for i2 in range(i + 1):
            lhs = C.lstrict_b if i2 == i else C.ones_b
            p.op("pe", lambda e, lhs=lhs, i2=i2, i=i: e.matmul(PS[3][:, 0:NE], lhs[:], maskb[:, i2, :],
                                                               start=(i2 == 0), stop=(i2 == i)),
                 reads=[rt_t[i2], C.t], writes=[PS_t[3]])
        p.op("dve", lambda e, i=i: e.scalar_tensor_tensor(posm[:, i, :], PS[3][:, 0:NE], 1.0, gates[:, i, :],
                                                          ALU.add, ALU.mult),
             reads=[PS_t[3], rt_t[i]], writes=[rt_t[i]])
        p.op("dve", lambda e, i=i: e.tensor_scalar(posm[:, i, :], posm[:, i, :], -1.0, None, ALU.add),
             reads=[rt_t[i]], writes=[rt_t[i]])
        p.op("dve", lambda e, r=r, i=i: e.tensor_tensor(r[:, 48:80], r[:, 48:80], gates[:, i, :], ALU.mult),
             reads=[r_t, rt_t[i]], writes=[r_t])
        p.op("dve", lambda e, r=r: e.reduce_sum(r[:, 41:42], r[:, 48:80], AX.X), reads=[r_t], writes=[r_t])
        p.op("dve", lambda e, r=r: e.reciprocal(r[:, 41:42], r[:, 41:42]), reads=[r_t], writes=[r_t])
        p.op("dve", lambda e, r=r, i=i: e.tensor_scalar(gates[:, i, :], r[:, 48:80], r[:, 41:42], None, ALU.mult),
             reads=[r_t], writes=[rt_t[i]])
        p.op("dve", lambda e, i=i: e.tensor_copy(ghl[:, i, :, 0], gates[:, i, :]), reads=[rt_t[i]], writes=[rt_t[i]])
        p.op("dve", lambda e, i=i, r=r: e.tensor_tensor(r[:, 48:80], gates[:, i, :], ghl[:, i, :, 0], ALU.subtract),
             reads=[rt_t[i], r_t], writes=[r_t])
        p.op("dve", lambda e, i=i, r=r: e.tensor_copy(ghl[:, i, :, 1], r[:, 48:80]), reads=[r_t], writes=[rt_t[i]])
        p.op("pe", lambda e, i=i: e.transpose(PS[6][0:NE, 0:128], gates[:, i, :], C.ident_f[:]),
             reads=[rt_t[i], C.t], writes=[PS_t[6]])
        p.op("act", lambda e, b=b: e.copy(gT[b][:], PS[6][0:NE, 0:128]), reads=[PS_t[6]], writes=[gT_t[b]])
        for dh in range(2):
            p.op("pe", lambda e, b=b, dh=dh: e.matmul(PS[4 + dh][:], gT[b][:], bd[:, dh * 512:(dh + 1) * 512],
                                                      start=True, stop=True),
                 reads=[gT_t[b], wr_t], writes=[PS_t[4 + dh]])
            p.op("dve", lambda e, x=x, i=i, dh=dh: e.scalar_tensor_tensor(
                acc[:, i, dh * 512:(dh + 1) * 512], x[:, dh * 512:(dh + 1) * 512], ALPHA, PS[4 + dh][:],
                ALU.mult, ALU.add), reads=[x_t, PS_t[4 + dh]], writes=[acc_t[i]])

    p.end_phase()
    p.begin_phase()
    S2 = [p.sbuf("S%d" % i, [128, NCH, CAP], BF16) for i in range(2)]
    S2_t = [[T() for _ in range(NCH)] for _ in range(2)]
    XgT = p.sbuf("XgT", [128, 8, CAP], BF16)
    XgT_t = [T() for _ in range(8)]
    actT = p.sbuf("actT", [128, 8, CAP], BF16)
    actT_t = [T() for _ in range(8)]
    Yg = p.sbuf("Yg", [128, NJC, D], BF16)
    Yg_t = [T() for _ in range(NJC)]
    gsl = p.sbuf("gsl", [128, NJC], F32); gsl_t = T()
    NWG = 3
    wg = [p.sbuf("wg%d" % i, [128, 8, 2, 256], BF16) for i in range(NWG)]
    wg_t = [T() for _ in range(NWG)]
    NWD = 2
    wdt = [p.sbuf("wd%d" % i, [128, 8, 512], BF16) for i in range(NWD)]
    wdt_t = [T() for _ in range(NWD)]
    NTMP = 3
    tg = [p.sbuf("tg%d" % i, [128, CAP], F32) for i in range(NTMP)]
    tsg = [p.sbuf("tsg%d" % i, [128, CAP], F32) for i in range(NTMP)]
    tu = [p.sbuf("tu%d" % i, [128, CAP], F32) for i in range(NTMP)]
    tmp_t = [T() for _ in range(NTMP)]
    NST = 4
    ST = [p.sbuf("ST%d" % i, [128, NJC, 128], BF16) for i in range(NST)]
    ST_t = [T() for _ in range(NST)]
    nwg = 0
    nwd = 0
    ntmp = 0
    nst = 0
    ndn = 0
    nsc = 0
    exlist = list(order if order is not None else range(NE))

    def build_S(n):
        ex_ = exlist[n]
        Sb, Sb_t = S2[n % 2], S2_t[n % 2]
        for i in range(NCH):
            p.op("dve", lambda e, i=i, ex_=ex_, Sb=Sb: e.tensor_scalar(Sb[:, i, :], C.iota_c[:], posm[:, i, ex_:ex_ + 1], None,
                                                                    ALU.is_equal),
                 reads=[rt_t[i], C.t], writes=[Sb_t[i]])

    build_S(0)
    for n_ex, ex in enumerate(exlist):
        S, S_t = S2[n_ex % 2], S2_t[n_ex % 2]
        for dc in range(8):
            ps, ps_t = PS[dc % 2], PS_t[dc % 2]
            for i in range(NCH):
                p.op("pe", lambda e, ps=ps, i=i, dc=dc, S=S: e.matmul(ps[:, 0:CAP], xtok[:, i, dc * 128:(dc + 1) * 128],
                                                                S[:, i, :], start=(i == 0), stop=(i == NCH - 1)),
                     reads=[xtok_t[i], S_t[i]], writes=[ps_t])
            p.op("act", lambda e, ps=ps, dc=dc: e.copy(XgT[:, dc, :], ps[:, 0:CAP]), reads=[ps_t], writes=[XgT_t[dc]])
        if n_ex + 1 < len(exlist):
            build_S(n_ex + 1)
        for jc in range(NJC):
            for i in range(NCH):
                p.op("pe", lambda e, i=i, jc=jc, ex=ex, S=S: e.matmul(PS[6][:, 2 * jc:2 * jc + 2],
                                                                 S[:, i, jc * 128:(jc + 1) * 128], ghl[:, i, ex, :],
                                                                 start=(i == 0), stop=(i == NCH - 1)),
                     reads=[S_t[i], rt_t[i]], writes=[PS_t[6]])
        p.op("dve", lambda e: e.reduce_sum(gsl[:], PS[6][:, 0:2 * NJC].rearrange("p (j two) -> p j two", two=2), AX.X),
             reads=[PS_t[6]], writes=[gsl_t])
        for fp in range(4):
            w, w_t = wg[nwg % NWG], wg_t[nwg % NWG]
            nwg += 1
            for two in range(2):
                wsrc, wsrc_t = wgu_of(ex)
                src = wsrc.rearrange("(k p) f -> p k f", p=128)[:, :, two * D + fp * 256:two * D + (fp + 1) * 256]
                p.dma("pool", lambda e, w=w, src=src, two=two: e.dma_start(out=w[:, :, two, :], in_=src), reads=[wsrc_t], writes=[w_t])
            for q in range(2):
                fc = fp * 2 + q
                pgi, pui = 2 + 2 * (fc % 2), 3 + 2 * (fc % 2)
                for kc in range(8):
                    p.op("pe", lambda e, w=w, kc=kc, q=q, pgi=pgi: e.matmul(PS[pgi][:, 0:CAP], w[:, kc, 0, q * 128:(q + 1) * 128],
                                                                        XgT[:, kc, :], start=(kc == 0), stop=(kc == 7)),
                         reads=[w_t, XgT_t[kc]], writes=[PS_t[pgi]])
                for kc in range(8):
                    p.op("pe", lambda e, w=w, kc=kc, q=q, pui=pui: e.matmul(PS[pui][:, 0:CAP], w[:, kc, 1, q * 128:(q + 1) * 128],
                                                                        XgT[:, kc, :], start=(kc == 0), stop=(kc == 7)),
                         reads=[w_t, XgT_t[kc]], writes=[PS_t[pui]])
                k = ntmp % NTMP
                ntmp += 1
                g_, sg_, u_, tt = tg[k], tsg[k], tu[k], tmp_t[k]
                p.op("dve", lambda e, g_=g_, fc=fc, ex=ex, pgi=pgi: e.tensor_scalar(g_[:], PS[pgi][:, 0:CAP], bguT[:, fc, ex:ex + 1],
                                                                                  SW_LIMIT, ALU.add, ALU.min),
                     reads=[PS_t[pgi], bguT_t], writes=[tt])
                p.op("act", lambda e, g_=g_, sg_=sg_: e.activation(sg_[:], g_[:], ACTF.Sigmoid, scale=SW_ALPHA),
                     reads=[tt], writes=[tt])
                p.op("dve", lambda e, u_=u_, fc=fc, ex=ex, pui=pui: e.tensor_scalar(u_[:], PS[pui][:, 0:CAP],
                                                                                  bguT[:, 8 + fc, ex:ex + 1], SW_LIMIT,
                                                                                  ALU.add, ALU.min),
                     reads=[PS_t[pui], bguT_t], writes=[tt])
                p.op("pool", lambda e, u_=u_: e.tensor_scalar(u_[:], u_[:], -SW_LIMIT, 1.0, ALU.max, ALU.add),
                     reads=[tt], writes=[tt])
                p.op("pool", lambda e, g_=g_, sg_=sg_: e.tensor_tensor(g_[:], g_[:], sg_[:], ALU.mult),
                     reads=[tt], writes=[tt])
                p.op("pool", lambda e, g_=g_, u_=u_, fc=fc: e.tensor_tensor(actT[:, fc, :], g_[:], u_[:], ALU.mult),
                     reads=[tt], writes=[actT_t[fc]])
        for dh in range(2):
            w, w_t = wdt[nwd % NWD], wdt_t[nwd % NWD]
            nwd += 1
            wsrc, wsrc_t = wd_of(ex)
            src = wsrc.rearrange("(k p) n -> p k n", p=128)[:, :, dh * 512:(dh + 1) * 512]
            p.dma("pool", lambda e, w=w, src=src: e.dma_start(out=w[:], in_=src), reads=[wsrc_t], writes=[w_t])
            for jc in range(NJC):
                ps, ps_t = PS[ndn % 2], PS_t[ndn % 2]
                ndn += 1
                for kc in range(8):
                    p.op("pe", lambda e, ps=ps, w=w, kc=kc, jc=jc: e.matmul(ps[:], actT[:, kc, jc * 128:(jc + 1) * 128],
                                                                        w[:, kc, :], start=(kc == 0), stop=(kc == 7)),
                         reads=[actT_t[kc], w_t], writes=[ps_t])
                p.op("act", lambda e, ps=ps, jc=jc, dh=dh: e.activation(Yg[:, jc, dh * 512:(dh + 1) * 512], ps[:],
                                                                       ACTF.Copy, scale=gsl[:, jc:jc + 1]),
                     reads=[ps_t, gsl_t], writes=[Yg_t[jc]])
        for i in range(NCH):
            st, st_t = ST[nst % NST], ST_t[nst % NST]
            nst += 1
            psb = PS[7][:].bitcast(BF16)
            for jc in range(NJC):
                p.op("pe", lambda e, i=i, jc=jc, psb=psb, S=S: e.transpose(psb[:, jc * 128:(jc + 1) * 128],
                                                                     S[:, i, jc * 128:(jc + 1) * 128], C.ident_b[:]),
                     reads=[S_t[i], C.t], writes=[PS_t[7]])
            p.op("act", lambda e, st=st, psb=psb: e.copy(st[:], psb[:, 0:NJC * 128].rearrange("p (j t) -> p j t", j=NJC)),
                 reads=[PS_t[7]], writes=[st_t])
            for dh in range(2):
                ps, ps_t = PS[2 + nsc % 4], PS_t[2 + nsc % 4]
                nsc += 1
                for jc in range(NJC):
                    p.op("pe", lambda e, ps=ps, st=st, jc=jc, dh=dh: e.matmul(ps[:], st[:, jc, :],
                                                                          Yg[:, jc, dh * 512:(dh + 1) * 512],
                                                                          start=(jc == 0), stop=(jc == NJC - 1)),
                         reads=[st_t, Yg_t[jc]], writes=[ps_t])
                p.op("dve", lambda e, ps=ps, i=i, dh=dh: e.tensor_tensor(acc[:, i, dh * 512:(dh + 1) * 512],
                                                                       acc[:, i, dh * 512:(dh + 1) * 512], ps[:], ALU.add),
                     reads=[ps_t, acc_t[i]], writes=[acc_t[i]])

    p.end_phase()
    p.begin_phase()
    gt = p.sbuf("lng", [128, D], F32)
    bt = p.sbuf("lnb", [128, D], F32)
    gb_t = T()
    p.dma("sp", lambda e: e.dma_start(out=gt[:], in_=bcast_rows(lng_d)), writes=[gb_t])
    p.dma("sp", lambda e: e.dma_start(out=bt[:], in_=bcast_rows(lnb_d)), writes=[gb_t])
    lst = [p.sbuf("lst%d" % i, [128, 16], F32) for i in range(2)]
    lst_t = [T() for _ in range(2)]
    for i in range(NCH):
        emit_ln(p, acc[:, i, :], acc_t[i], gt, bt, gb_t, lst[i % 2], lst_t[i % 2], eng2="dve")
        p.dma("sp", lambda e, i=i: e.dma_start(out=h2_d[i * 128:(i + 1) * 128, :], in_=acc[:, i, :]),
              reads=[acc_t[i]], writes=[h2_dt])
    p.end_phase()
    p.end_phase()


def build_moe_only():
    nc = bass.Bass("TRN2", target_bir_lowering=False)
    h1 = nc.dram_tensor("h1", [NTOK, D], F32, kind="ExternalInput").ap()
    wr = nc.dram_tensor("wr", [D, NE], F32, kind="ExternalInput").ap()
    br = nc.dram_tensor("br", [NE], F32, kind="ExternalInput").ap()
    wgu = nc.dram_tensor("wgu", [NE, D, 2 * D], F32, kind="ExternalInput").ap()
    bgu = nc.dram_tensor("bgu", [NE, 2 * D], F32, kind="ExternalInput").ap()
    wd = nc.dram_tensor("wd", [NE, D, D], F32, kind="ExternalInput").ap()
    bd = nc.dram_tensor("bd", [NE, D], F32, kind="ExternalInput").ap()
    lng = nc.dram_tensor("lng", [D], F32, kind="ExternalInput").ap()
    lnb = nc.dram_tensor("lnb", [D], F32, kind="ExternalInput").ap()
    h2 = nc.dram_tensor("h2", [NTOK, D], F32, kind="ExternalOutput").ap()
    p = Prog(nc)
    C = Consts(p)
    wt_, wt2_ = T(), T()
    emit_moe(p, C, h1, T(), h2, T(), wr, br, lambda ex: (wgu[ex], wt_), bgu, lambda ex: (wd[ex], wt2_), bd, lng, lnb)
    p.finish()
    return nc


def emit_hT(p, C, PS, PS_t, h_d, h_dt, halo_d, halo_dt, hT, hT_t):
    p.begin_phase()
    xin = [p.sbuf("hx%d" % i, [128, D], F32) for i in range(2)]
    xin_t = [T() for _ in range(2)]
    hl = p.sbuf("hl", [32, D], F32); hl_t = T()
    p.dma("sp", lambda e: e.dma_start(out=hl[:], in_=halo_d), reads=[halo_dt], writes=[hl_t])
    for half in range(2):
        ps, ps_t = PS[half], PS_t[half]
        for q in range(4):
            dc = half * 4 + q
            p.op("pe", lambda e, dc=dc, q=q, ps=ps: e.transpose(ps[:, q * 32:(q + 1) * 32], hl[:, dc * 128:(dc + 1) * 128],
                                                              C.ident_f[0:32, 0:32]), reads=[hl_t, C.t], writes=[ps_t])
        p.op("dve", lambda e, ps=ps, half=half: e.tensor_copy(hT[:, half * 4:(half + 1) * 4, 0:32],
                                                             ps[:, 0:128].rearrange("p (q t) -> p q t", q=4)),
             reads=[ps_t], writes=[hT_t])
    for i in range(NCH):
        b = i % 2
        x, x_t = xin[b], xin_t[b]
        p.dma("sp", lambda e, x=x, i=i: e.dma_start(out=x[:], in_=h_d[i * 128:(i + 1) * 128, :]), reads=[h_dt], writes=[x_t])
        for half in range(2):
            ps, ps_t = PS[2 + half], PS_t[2 + half]
            for q in range(4):
                dc = half * 4 + q
                p.op("pe", lambda e, x=x, dc=dc, q=q, ps=ps: e.transpose(ps[:, q * 128:(q + 1) * 128],
                                                                     x[:, dc * 128:(dc + 1) * 128], C.ident_f[:]),
                     reads=[x_t, C.t], writes=[ps_t])
            if half == 0:
                p.op("dve", lambda e, ps=ps, i=i: e.tensor_copy(hT[:, 0:4, 32 + i * 128:32 + (i + 1) * 128],
                                                              ps[:].rearrange("p (q t) -> p q t", q=4)),
                     reads=[ps_t], writes=[hT_t])
            else:
                p.op("act", lambda e, ps=ps, i=i: e.copy(hT[:, 4:8, 32 + i * 128:32 + (i + 1) * 128],
                                                        ps[:].rearrange("p (q t) -> p q t", q=4)),
                     reads=[ps_t], writes=[hT_t])
    p.end_phase()


def load_rowsT(p, C, ps, ps_t, src_d, nrows, dst, dst_t, name):
    raw = p.sbuf(name, [nrows, D], F32); raw_t = T()
    p.dma("sp", lambda e: e.dma_start(out=raw[:], in_=src_d), writes=[raw_t])
    for c in range(8):
        p.op("pe", lambda e, c=c: e.transpose(ps[:, 0:nrows], raw[:, c * 128:(c + 1) * 128], C.ident_f[0:nrows, 0:nrows]),
             reads=[raw_t, C.t], writes=[ps_t])
        p.op("dve", lambda e, c=c: e.tensor_copy(dst[:, c, :], ps[:, 0:nrows]), reads=[ps_t], writes=[dst_t])


CONF_K = 31
TT = [(0, 32)] + [(32 + i * 512, 512) for i in range(4)]


def emit_conformer(p, C, h_d, h_dt, halo_d, halo_dt, hasprev_d, h1_d, h1_dt,
                   w1_d, w1_dt, b1_d, wdw_d, bdw_d, lng_d, lnb_d, w2_d, w2_dt, b2_d, mg_d, mb_d):
    p.begin_phase()
    PS = [p.psum("cps%d" % i, [128, 512], F32) for i in range(8)]
    PS_t = [T() for _ in range(8)]
    vT = p.sbuf("vT", [128, 8, NTOK], F32)
    vT_t = [T() for _ in range(8)]
    prm = p.sbuf("cprm", [128, 8, 8], F32); prm_t = T()
    wdwT = p.sbuf("wdwT", [128, 8, CONF_K], F32); wdwT_t = T()
    hp = p.sbuf("hasprev", [128, 1], F32)
    p.dma("sp", lambda e: e.dma_start(out=hp[:], in_=hasprev_d.partition_broadcast(128)), writes=[prm_t])
    p.begin_phase()
    praw = p.sbuf("praw", [8, D], F32); praw_t = T()
    srcs = [b1_d[0:D], b1_d[D:2 * D], bdw_d, lng_d, lnb_d]
    for j, s_ in enumerate(srcs):
        p.dma("sp", lambda e, j=j, s_=s_: e.dma_start(out=praw[j:j + 1, :], in_=s_.rearrange("(o n) -> o n", o=1)), writes=[praw_t])
    for c in range(8):
        p.op("pe", lambda e, c=c: e.transpose(PS[7][:, 0:5], praw[0:5, c * 128:(c + 1) * 128], C.ident_f[0:5, 0:5]),
             reads=[praw_t, C.t], writes=[PS_t[7]])
        p.op("dve", lambda e, c=c: e.tensor_copy(prm[:, c, 0:5], PS[7][:, 0:5]), reads=[PS_t[7]], writes=[prm_t])
    load_rowsT(p, C, PS[6], PS_t[6], wdw_d, CONF_K, wdwT, wdwT_t, "wdwraw")
    hT = p.sbuf("hT", [128, 8, 32 + NTOK], BF16); hT_t = T()
    emit_hT(p, C, PS, PS_t, h_d, h_dt, halo_d, halo_dt, hT, hT_t)
    w1t = [p.sbuf("w1t%d" % i, [128, 8, 2, 256], BF16) for i in range(2)]
    w1t_t = [T() for _ in range(2)]
    ut = [p.sbuf("ut%d" % i, [128, 32 + NTOK], F32) for i in range(2)]
    ut_t = [T() for _ in range(2)]
    sg = [p.sbuf("sg%d" % i, [128, 512], F32) for i in range(2)]
    sg_t = [T() for _ in range(2)]
    nsg = 0
    ptmp = p.sbuf("ptmp", [128, 512], F32); ptmp_t = T()
    for cp in range(4):
        w, w_t = w1t[cp % 2], w1t_t[cp % 2]
        for two in range(2):
            src = w1_d.rearrange("(k p) f -> p k f", p=128)[:, :, two * D + cp * 256:two * D + (cp + 1) * 256]
            p.dma("pool", lambda e, w=w, src=src, two=two: e.dma_start(out=w[:, :, two, :], in_=src), reads=[w1_dt], writes=[w_t])
        for q in range(2):
            cc = cp * 2 + q
            u, u_t = ut[cc % 2], ut_t[cc % 2]
            for ti, (c0, n) in enumerate(TT):
                pa, pa_t = PS[(2 * ti) % 6], PS_t[(2 * ti) % 6]
                pg, pg_t = PS[(2 * ti + 1) % 6], PS_t[(2 * ti + 1) % 6]
                for k in range(8):
                    p.op("pe", lambda e, pa=pa, w=w, k=k, q=q, c0=c0, n=n: e.matmul(
                        pa[:, 0:n], w[:, k, 0, q * 128:(q + 1) * 128], hT[:, k, c0:c0 + n], start=(k == 0), stop=(k == 7)),
                        reads=[w_t, hT_t], writes=[pa_t])
                for k in range(8):
                    p.op("pe", lambda e, pg=pg, w=w, k=k, q=q, c0=c0, n=n: e.matmul(
                        pg[:, 0:n], w[:, k, 1, q * 128:(q + 1) * 128], hT[:, k, c0:c0 + n], start=(k == 0), stop=(k == 7)),
                        reads=[w_t, hT_t], writes=[pg_t])
                s_, s_t = sg[nsg % 2], sg_t[nsg % 2]
                nsg += 1
                p.op("act", lambda e, s_=s_, pg=pg, n=n, cc=cc: e.activation(s_[:, 0:n], pg[:, 0:n], ACTF.Sigmoid,
                                                                           bias=prm[:, cc, 1:2]),
                     reads=[pg_t, prm_t], writes=[s_t])
                p.op("dve", lambda e, u=u, pa=pa, s_=s_, c0=c0, n=n, cc=cc: e.scalar_tensor_tensor(
                    u[:, c0:c0 + n], pa[:, 0:n], prm[:, cc, 0:1], s_[:, 0:n], ALU.add, ALU.mult),
                    reads=[pa_t, s_t, prm_t], writes=[u_t])
            p.op("dve", lambda e, u=u: e.tensor_scalar(u[:, 0:32], u[:, 0:32], hp[:, 0:1], None, ALU.mult),
                 reads=[u_t, prm_t], writes=[u_t])
            TS = 1536
            vt = T()
            for k in range(CONF_K):
                off = 32 - (CONF_K - 1) + k
                if k == 0:
                    p.op("dve", lambda e, u=u, cc=cc, off=off: e.tensor_scalar(
                        vT[:, cc, 0:TS], u[:, off:off + TS], wdwT[:, cc, 0:1], prm[:, cc, 2:3], ALU.mult, ALU.add),
                        reads=[u_t, wdwT_t, prm_t], writes=[vt])
                else:
                    p.op("dve", lambda e, u=u, cc=cc, off=off, k=k: e.scalar_tensor_tensor(
                        vT[:, cc, 0:TS], u[:, off:off + TS], wdwT[:, cc, k:k + 1], vT[:, cc, 0:TS], ALU.mult, ALU.add),
                        reads=[u_t, wdwT_t, vt], writes=[vt])
            vt2 = T()
            n2 = NTOK - TS
            for k in range(CONF_K):
                off = 32 + TS - (CONF_K - 1) + k
                if k == 0:
                    p.op("pool", lambda e, u=u, cc=cc, off=off: e.tensor_scalar(
                        vT[:, cc, TS:NTOK], u[:, off:off + n2], wdwT[:, cc, 0:1], prm[:, cc, 2:3], ALU.mult, ALU.add),
                        reads=[u_t, wdwT_t, prm_t], writes=[vt2])
                else:
                    p.op("pool", lambda e, u=u, cc=cc, off=off, k=k: e.tensor_scalar(
                        ptmp[:], u[:, off:off + n2], wdwT[:, cc, k:k + 1], None, ALU.mult),
                        reads=[u_t, wdwT_t], writes=[ptmp_t])
                    p.op("pool", lambda e, cc=cc: e.tensor_tensor(vT[:, cc, TS:NTOK], vT[:, cc, TS:NTOK], ptmp[:], ALU.add),
                         reads=[ptmp_t, vt2], writes=[vt2])
    p.end_phase()
    p.begin_phase()
    wT = p.sbuf("wT", [128, 8, NTOK], BF16)
    wT_t = [T() for _ in range(4)]
    ones_f = p.sbuf("ones_f", [128, 128], F32); ones_t = T()
    p.op("pool", lambda e: e.memset(ones_f[:], 1.0), writes=[ones_t])
    w2 = p.sbuf("w2", [128, 8, D], BF16); w2_t = T()
    for half in range(2):
        p.dma("pool", lambda e, half=half: e.dma_start(out=w2[:, :, half * 512:(half + 1) * 512],
                                                      in_=w2_d.rearrange("(k p) n -> p k n", p=128)[:, :, half * 512:(half + 1) * 512]),
              reads=[w2_dt], writes=[w2_t])
    b2B = p.sbuf("b2B", [128, D], F32)
    mgB = p.sbuf("mgB", [128, D], F32)
    mbB = p.sbuf("mbB", [128, D], F32)
    gb_t = T()
    p.dma("sp", lambda e: e.dma_start(out=b2B[:], in_=b2_d.partition_broadcast(128)), writes=[gb_t])
    p.dma("sp", lambda e: e.dma_start(out=mgB[:], in_=mg_d.partition_broadcast(128)), writes=[gb_t])
    p.dma("sp", lambda e: e.dma_start(out=mbB[:], in_=mb_d.partition_broadcast(128)), writes=[gb_t])
    sq = [p.sbuf("sq%d" % i, [128, 512], F32) for i in range(2)]
    sq_t = [T() for _ in range(2)]
    mu = p.sbuf("mu", [128, 512], F32)
    rstd = p.sbuf("rstd", [128, 512], F32)
    st_t = T()
    tmp = [p.sbuf("lt%d" % i, [128, 512], F32) for i in range(2)]
    tmp_t = [T() for _ in range(2)]
    nsq = 0
    for tt in range(4):
        c0 = tt * 512
        for cc in range(8):
            p.op("pe", lambda e, cc=cc, c0=c0: e.matmul(PS[0][:], ones_f[:], vT[:, cc, c0:c0 + 512], start=(cc == 0), stop=(cc == 7)),
                 reads=[ones_t, vT_t[cc]], writes=[PS_t[0]])
        for cc in range(8):
            s_, s_t = sq[nsq % 2], sq_t[nsq % 2]
            nsq += 1
            p.op("act", lambda e, s_=s_, cc=cc, c0=c0: e.activation(s_[:], vT[:, cc, c0:c0 + 512], ACTF.Square),
                 reads=[vT_t[cc]], writes=[s_t])
            p.op("pe", lambda e, s_=s_, cc=cc: e.matmul(PS[1][:], ones_f[:], s_[:], start=(cc == 0), stop=(cc == 7)),
                 reads=[ones_t, s_t], writes=[PS_t[1]])
        p.op("dve", lambda e: e.tensor_scalar(mu[:], PS[0][:], 1.0 / D, None, ALU.mult), reads=[PS_t[0]], writes=[st_t])
        p.op("dve", lambda e: e.tensor_tensor(rstd[:], mu[:], mu[:], ALU.mult), reads=[st_t], writes=[st_t])
        p.op("dve", lambda e: e.scalar_tensor_tensor(rstd[:], PS[1][:], 1.0 / D, rstd[:], ALU.mult, ALU.subtract),
             reads=[PS_t[1], st_t], writes=[st_t])
        p.op("act", lambda e: e.activation(rstd[:], rstd[:], ACTF.Sqrt, bias=LN_EPS), reads=[st_t], writes=[st_t])
        p.op("dve", lambda e: e.reciprocal(rstd[:], rstd[:]), reads=[st_t], writes=[st_t])
        for cc in range(8):
            t_, t_t = tmp[cc % 2], tmp_t[cc % 2]
            p.op("dve", lambda e, t_=t_, cc=cc, c0=c0: e.tensor_tensor(t_[:], vT[:, cc, c0:c0 + 512], mu[:], ALU.subtract),
                 reads=[vT_t[cc], st_t], writes=[t_t])
            p.op("dve", lambda e, t_=t_: e.tensor_tensor(t_[:], t_[:], rstd[:], ALU.mult), reads=[t_t, st_t], writes=[t_t])
            p.op("act", lambda e, t_=t_, cc=cc, c0=c0: e.activation(wT[:, cc, c0:c0 + 512], t_[:], ACTF.Silu,
                                                                  bias=prm[:, cc, 4:5], scale=prm[:, cc, 3:4]),
                 reads=[t_t, prm_t], writes=[wT_t[tt]])
    hx = [p.sbuf("chx%d" % i, [128, D], F32) for i in range(2)]
    hx_t = [T() for _ in range(2)]
    lst = [p.sbuf("clst%d" % i, [128, 16], F32) for i in range(2)]
    lst_t = [T() for _ in range(2)]
    for i in range(NCH):
        b = i % 2
        x, x_t = hx[b], hx_t[b]
        p.dma("sp", lambda e, x=x, i=i: e.dma_start(out=x[:], in_=h_d[i * 128:(i + 1) * 128, :]), reads=[h_dt], writes=[x_t])
        p.op("dve", lambda e, x=x: e.scalar_tensor_tensor(x[:], x[:], ALPHA, b2B[:], ALU.mult, ALU.add),
             reads=[x_t, gb_t], writes=[x_t])
        for dh in range(2):
            ps, ps_t = PS[2 + dh], PS_t[2 + dh]
            for cc in range(8):
                p.op("pe", lambda e, ps=ps, cc=cc, i=i, dh=dh: e.matmul(ps[:], wT[:, cc, i * 128:(i + 1) * 128],
                                                                    w2[:, cc, dh * 512:(dh + 1) * 512],
                                                                    start=(cc == 0), stop=(cc == 7)),
                     reads=[wT_t[i // 4], w2_t], writes=[ps_t])
            p.op("dve", lambda e, ps=ps, x=x, dh=dh: e.tensor_tensor(x[:, dh * 512:(dh + 1) * 512],
                                                                   x[:, dh * 512:(dh + 1) * 512], ps[:], ALU.add),
                 reads=[ps_t, x_t], writes=[x_t])
        emit_ln(p, x[:], x_t, mgB, mbB, gb_t, lst[b], lst_t[b], eng2="pool")
        p.dma("sp", lambda e, x=x, i=i: e.dma_start(out=h1_d[i * 128:(i + 1) * 128, :], in_=x[:]), reads=[x_t], writes=[h1_dt])
    p.end_phase()
    p.end_phase()


def build_conf_only():
    nc = bass.Bass("TRN2", target_bir_lowering=False)
    def inp(name, shape):
        return nc.dram_tensor(name, list(shape), F32, kind="ExternalInput").ap()
    h = inp("h", [NTOK, D]); halo = inp("halo", [32, D]); hasprev = inp("hasprev", [1])
    w1 = inp("w1", [D, 2 * D]); b1 = inp("b1", [2 * D]); wdw = inp("wdw", [CONF_K, D]); bdw = inp("bdw", [D])
    lng = inp("lng", [D]); lnb = inp("lnb", [D]); w2 = inp("w2", [D, D]); b2 = inp("b2", [D])
    mg = inp("mg", [D]); mb = inp("mb", [D])
    h1 = nc.dram_tensor("h1", [NTOK, D], F32, kind="ExternalOutput").ap()
    p = Prog(nc)
    C = Consts(p)
    emit_conformer(p, C, h, T(), halo, T(), hasprev, h1, T(), w1, T(), b1, wdw, bdw, lng, lnb, w2, T(), b2, mg, mb)
    p.finish()
    return nc


def bc_last(ap, n):
    return bass.AP(ap.tensor, ap.offset, [list(x) for x in ap.ap] + [[0, n]])


def bc_mid(ap, n):
    l = [list(x) for x in ap.ap]
    return bass.AP(ap.tensor, ap.offset, [l[0], [0, n]] + l[1:])


def emit_allgather(p, src_d, src_t, dst_d, dst_t):
    p.coll(lambda e: e.collective_compute("AllGather", ALU.bypass, replica_groups=[list(range(8))],
                                          ins=[src_d.opt()], outs=[dst_d.opt()]),
           reads=[src_t], writes=[dst_t])


def emit_halo(p, C, nc, h_d, h_dt, sel_d, halo_d, halo_dt, tag):
    src = nc.dram_tensor("halo_src_" + tag, [32, D], F32).ap()
    gat = nc.dram_tensor("halo_gat_" + tag, [256, D], F32).ap()
    src_t, gat_t = T(), T()
    p.dma("sp", lambda e: e.dma_start(out=src, in_=h_d[NTOK - 32:NTOK, :]), reads=[h_dt], writes=[src_t])
    emit_allgather(p, src, src_t, gat, gat_t)
    p.begin_phase()
    ps = [p.psum("hps%d" % i, [128, 512], F32) for i in range(2)]
    ps_t = [T(), T()]
    g = p.sbuf("hg", [128, 2, D], F32); g_t = T()
    sel = p.sbuf("hsel", [128, 2, 32], F32)
    ho = p.sbuf("hout", [32, D], F32); ho_t = T()
    p.dma("sp", lambda e: e.dma_start(out=g[:], in_=gat.rearrange("(k p) d -> p k d", p=128)), reads=[gat_t], writes=[g_t])
    p.dma("sp", lambda e: e.dma_start(out=sel[:], in_=sel_d.rearrange("(k p) j -> p k j", p=128)), writes=[g_t])
    for dh in range(2):
        for k in range(2):
            p.op("pe", lambda e, dh=dh, k=k: e.matmul(ps[dh][0:32, :], sel[:, k, :], g[:, k, dh * 512:(dh + 1) * 512],
                                                      start=(k == 0), stop=(k == 1)), reads=[g_t], writes=[ps_t[dh]])
        p.op("dve", lambda e, dh=dh: e.tensor_copy(ho[:, dh * 512:(dh + 1) * 512], ps[dh][0:32, :]), reads=[ps_t[dh]], writes=[ho_t])
    p.dma("sp", lambda e: e.dma_start(out=halo_d, in_=ho[:]), reads=[ho_t], writes=[halo_dt])
    p.end_phase()


NH = 16
HP = 64
COL_Z = 3 * D
COL_XBC = 4 * D
COL_DT = 4 * D + 1536


def emit_even(p, C, nc, tag, h_d, h_dt, halo_d, halo_dt, h1_d, h1_dt, win_d, win_dt, wout_d, wout_dt,
              scw_d, cw_d, cb_d, dtb_d, alog_d, dsk_d, nw_d, mg_d, mb_d, before_d, between_d):
    winv = win_d.rearrange("(k p) f -> p k f", p=128)
    woutv = wout_d.rearrange("(k p) f -> p k f", p=128)
    hacc_d = nc.dram_tensor("hacc_" + tag, [NTOK, D], F32).ap(); hacc_dt = T()
    st_src = nc.dram_tensor("stsrc_" + tag, [128, D + NH], F32).ap(); st_src_t = T()
    st_gat = nc.dram_tensor("stgat_" + tag, [8 * 128, D + NH], F32).ap(); st_gat_t = T()

    p.begin_phase()
    PS = [p.psum("xps%d" % i, [128, 512], F32) for i in range(8)]
    PS_t = [T() for _ in range(8)]
    hT = p.sbuf("xhT", [128, 8, 32 + NTOK], BF16); hT_t = T()
    emit_hT(p, C, PS, PS_t, h_d, h_dt, halo_d, halo_dt, hT, hT_t)
    p.begin_phase()
    scT = p.sbuf("scT", [128, 8, 3], F32); scT_t = T()
    load_rowsT(p, C, PS[7], PS_t[7], scw_d, 3, scT, scT_t, "scraw")
    yscT = p.sbuf("yscT", [128, 8, NTOK], BF16); yscT_t = [T() for _ in range(8)]
    wo1 = p.sbuf("wo1", [128, 8, D], BF16); wo1_t = T()
    for half in range(2):
        p.dma("pool", lambda e, half=half: e.dma_start(out=wo1[:, :, half * 512:(half + 1) * 512],
                                                      in_=woutv[:, 0:8, half * 512:(half + 1) * 512]), reads=[wout_dt], writes=[wo1_t])
    w3 = [p.sbuf("w3_%d" % i, [128, 8, 3, 256], BF16) for i in range(2)]
    w3_t = [T(), T()]
    pp = [p.sbuf("pp%d" % i, [128, 32 + NTOK], F32) for i in range(2)]
    pp_t = [T(), T()]
    po = [p.sbuf("po%d" % i, [128, 32 + NTOK], F32) for i in range(2)]
    po_t = [T(), T()]
    tp = [p.sbuf("tp%d" % i, [128, 512], F32) for i in range(2)]
    tp_t = [T(), T()]
    cc3 = [p.sbuf("cc3_%d" % i, [128, NTOK], F32) for i in range(2)]
    cc3_t = [T(), T()]
    ntp = 0
    nrot = 0
    for cp in range(4):
        w, w_t = w3[cp % 2], w3_t[cp % 2]
        for br in range(3):
            p.dma("pool", lambda e, w=w, br=br, cp=cp: e.dma_start(out=w[:, :, br, :], in_=winv[:, :, br * D + cp * 256:br * D + (cp + 1) * 256]),
                  reads=[win_dt], writes=[w_t])
        for q in range(2):
            cc = cp * 2 + q
            pp_, ppt = pp[cc % 2], pp_t[cc % 2]
            po_, pot = po[cc % 2], po_t[cc % 2]
            for ti, (c0, n) in enumerate(TT):
                banks = [(nrot + b) % 6 for b in range(3)]
                nrot += 3
                for br in range(3):
                    ps, ps_t = PS[banks[br]], PS_t[banks[br]]
                    for k in range(8):
                        p.op("pe", lambda e, ps=ps, w=w, k=k, q=q, br=br, c0=c0, n=n: e.matmul(
                            ps[:, 0:n], w[:, k, br, q * 128:(q + 1) * 128], hT[:, k, c0:c0 + n], start=(k == 0), stop=(k == 7)),
                            reads=[w_t, hT_t], writes=[ps_t])
                t_, t_t = tp[ntp % 2], tp_t[ntp % 2]
                ntp += 1
                p.op("act", lambda e, t_=t_, n=n, b1=banks[1]: e.copy(t_[:, 0:n], PS[b1][:, 0:n]), reads=[PS_t[banks[1]]], writes=[t_t])
                p.op("dve", lambda e, t_=t_, n=n, c0=c0, pp_=pp_, b0=banks[0]: e.tensor_tensor(pp_[:, c0:c0 + n], PS[b0][:, 0:n], t_[:, 0:n], ALU.mult),
                     reads=[PS_t[banks[0]], t_t], writes=[ppt])
                p.op("act", lambda e, n=n, c0=c0, po_=po_, b2=banks[2]: e.copy(po_[:, c0:c0 + n], PS[b2][:, 0:n]), reads=[PS_t[banks[2]]], writes=[pot])
            c_, c_t = cc3[cc % 2], cc3_t[cc % 2]
            for k in range(3):
                off = 32 - 2 + k
                if k == 0:
                    p.op("dve", lambda e, c_=c_, pp_=pp_, cc=cc, off=off: e.tensor_scalar(c_[:], pp_[:, off:off + NTOK], scT[:, cc, 0:1], None, ALU.mult),
                         reads=[ppt, scT_t], writes=[c_t])
                else:
                    p.op("dve", lambda e, c_=c_, pp_=pp_, cc=cc, off=off, k=k: e.scalar_tensor_tensor(
                        c_[:], pp_[:, off:off + NTOK], scT[:, cc, k:k + 1], c_[:], ALU.mult, ALU.add), reads=[ppt, scT_t, c_t], writes=[c_t])
            p.op("pool", lambda e, c_=c_, po_=po_, cc=cc: e.tensor_tensor(yscT[:, cc, :], c_[:], po_[:, 32:32 + NTOK], ALU.mult),
                 reads=[c_t, pot], writes=[yscT_t[cc]])
    hx = [p.sbuf("ehx%d" % i, [128, D], F32) for i in range(2)]
    hx_t = [T(), T()]
    for i in range(NCH):
        x, x_t = hx[i % 2], hx_t[i % 2]
        p.dma("sp", lambda e, x=x, i=i: e.dma_start(out=x[:], in_=h_d[i * 128:(i + 1) * 128, :]), reads=[h_dt], writes=[x_t])
        for dh in range(2):
            ps, ps_t = PS[6 + dh], PS_t[6 + dh]
            for cc in range(8):
                p.op("pe", lambda e, ps=ps, cc=cc, i=i, dh=dh: e.matmul(ps[:], yscT[:, cc, i * 128:(i + 1) * 128],
                                                                    wo1[:, cc, dh * 512:(dh + 1) * 512], start=(cc == 0), stop=(cc == 7)),
                     reads=[yscT_t[cc], wo1_t], writes=[ps_t])
            p.op("dve", lambda e, ps=ps, x=x, dh=dh: e.scalar_tensor_tensor(x[:, dh * 512:(dh + 1) * 512], x[:, dh * 512:(dh + 1) * 512],
                                                                          ALPHA, ps[:], ALU.mult, ALU.add), reads=[ps_t, x_t], writes=[x_t])
        p.dma("sp", lambda e, x=x, i=i: e.dma_start(out=hacc_d[i * 128:(i + 1) * 128, :], in_=x[:]), reads=[x_t], writes=[hacc_dt])
    p.end_phase()
    p.end_phase()

    p.begin_phase()
    PS = [p.psum("eps%d" % i, [128, 512], F32) for i in range(8)]
    PS_t = [T() for _ in range(8)]
    xs = p.sbuf("xs", [128, NCH, D], BF16); xs_t = [T() for _ in range(NCH)]
    btok = p.sbuf("btok", [128, NCH, 256], BF16); btok_t = [T() for _ in range(NCH)]
    BT = p.sbuf("BT", [128, 2, NTOK], BF16); BT_t = T()
    CT = p.sbuf("CT", [128, 2, NTOK], BF16); CT_t = T()
    dt = p.sbuf("dt", [128, NCH, NH], F32)
    da = p.sbuf("da", [128, NCH, NH], F32)
    Ea = p.sbuf("Ea", [128, NCH, NH], F32)
    dec = p.sbuf("dec", [128, NCH, NH], F32)
    wst = p.sbuf("wst", [128, NCH, NH], F32)
    dd_t = T()
    state = p.sbuf("state", [128, D], F32); state_t = T()
    state_b = p.sbuf("state_b", [128, D], BF16); state_bt = T()
    logD = p.sbuf("logD", [128, NH], F32); logD_t = T()
    U_f = p.sbuf("U_f", [128, 128], F32)
    Ls_f = p.sbuf("Ls_f", [128, 128], F32)
    ones_f = p.sbuf("eones_f", [128, 128], F32)
    cst_t = T()
    p.op("pool", lambda e: e.memset(ones_f[:], 1.0), writes=[cst_t])
    p.op("pool", lambda e: e.memset(U_f[:], 1.0), writes=[cst_t])
    p.op("pool", lambda e: e.affine_select(U_f[:], U_f[:], [[1, 128]], ALU.is_ge, 0.0, base=0, channel_multiplier=-1),
         reads=[cst_t], writes=[cst_t])
    p.op("pool", lambda e: e.memset(Ls_f[:], 1.0), writes=[cst_t])
    p.op("pool", lambda e: e.affine_select(Ls_f[:], Ls_f[:], [[-1, 128]], ALU.is_gt, 0.0, base=0, channel_multiplier=1),
         reads=[cst_t], writes=[cst_t])
    p.op("pool", lambda e: e.memset(state[:], 0.0), writes=[state_t])
    hp16 = p.sbuf("hp16", [128, 4, NH], F32); hp16_t = T()
    p.dma("sp", lambda e: e.dma_start(out=hp16[:, 0, :], in_=dtb_d.partition_broadcast(128)), writes=[hp16_t])
    p.dma("sp", lambda e: e.dma_start(out=hp16[:, 1, :], in_=alog_d.partition_broadcast(128)), writes=[hp16_t])
    p.dma("sp", lambda e: e.dma_start(out=hp16[:, 2, :], in_=dsk_d.partition_broadcast(128)), writes=[hp16_t])
    p.op("act", lambda e: e.activation(hp16[:, 1, :], hp16[:, 1, :], ACTF.Exp), reads=[hp16_t], writes=[hp16_t])
    p.op("dve", lambda e: e.tensor_scalar(hp16[:, 1, :], hp16[:, 1, :], -1.0, None, ALU.mult), reads=[hp16_t], writes=[hp16_t])
    xdd = [p.sbuf("xdd%d" % i, [128, D], BF16) for i in range(2)]
    xdd_t = [T(), T()]
    sz = p.sbuf("sz", [128, NCH, D], BF16); sz_t = [T() for _ in range(NCH)]

    def state_step(i):
        x_, x_t = xdd[i % 2], xdd_t[i % 2]
        p.op("dve", lambda e: e.tensor_tensor(x_[:].rearrange("p (h q) -> p h q", h=NH), xs[:, i, :].rearrange("p (h q) -> p h q", h=NH),
                                              bc_last(wst[:, i, :], HP), ALU.mult), reads=[xs_t[i], dd_t], writes=[x_t])
        for g in range(2):
            p.op("pe", lambda e, g=g: e.matmul(PS[5 + g][:], btok[:, i, g * 128:(g + 1) * 128], x_[:, g * 512:(g + 1) * 512],
                                               start=True, stop=True), reads=[btok_t[i], x_t], writes=[PS_t[5 + g]])
        p.op("dve", lambda e: e.tensor_tensor(state[:].rearrange("p (h q) -> p h q", h=NH), state[:].rearrange("p (h q) -> p h q", h=NH),
                                              bc_last(dec[:, i, :], HP), ALU.mult), reads=[state_t, dd_t], writes=[state_t])
        for g in range(2):
            p.op("dve", lambda e, g=g: e.tensor_tensor(state[:, g * 512:(g + 1) * 512], state[:, g * 512:(g + 1) * 512],
                                                       PS[5 + g][:], ALU.add), reads=[state_t, PS_t[5 + g]], writes=[state_t])
        p.op("act", lambda e: e.copy(state_b[:], state[:]), reads=[state_t], writes=[state_bt])

    p.begin_phase()
    hT = p.sbuf("ehT", [128, 8, 32 + NTOK], BF16); hT_t = T()
    emit_hT(p, C, PS, PS_t, h_d, h_dt, halo_d, halo_dt, hT, hT_t)
    p.begin_phase()
    wz = p.sbuf("wz", [128, 8, D], BF16); wz_t = T()
    for half in range(2):
        p.dma("pool", lambda e, half=half: e.dma_start(out=wz[:, :, half * 512:(half + 1) * 512],
                                                      in_=winv[:, :, COL_Z + half * 512:COL_Z + (half + 1) * 512]), reads=[win_dt], writes=[wz_t])
    for i in range(NCH):
        for dh in range(2):
            ps, ps_t = PS[(i * 2 + dh) % 4], PS_t[(i * 2 + dh) % 4]
            for k in range(8):
                p.op("pe", lambda e, ps=ps, k=k, i=i, dh=dh: e.matmul(ps[:], hT[:, k, 32 + i * 128:32 + (i + 1) * 128],
                                                                  wz[:, k, dh * 512:(dh + 1) * 512], start=(k == 0), stop=(k == 7)),
                     reads=[hT_t, wz_t], writes=[ps_t])
            p.op("act", lambda e, ps=ps, i=i, dh=dh: e.activation(sz[:, i, dh * 512:(dh + 1) * 512], ps[:], ACTF.Silu),
                 reads=[ps_t], writes=[sz_t[i]])
    p.end_phase()
    p.begin_phase()
    craw = p.sbuf("craw", [5, 1536], F32); craw_t = T()
    p.dma("sp", lambda e: e.dma_start(out=craw[0:4, :], in_=cw_d), writes=[craw_t])
    p.dma("sp", lambda e: e.dma_start(out=craw[4:5, :], in_=cb_d.rearrange("(o n) -> o n", o=1)), writes=[craw_t])
    cwT = p.sbuf("cwT", [128, 12, 8], F32); cwT_t = T()
    for mc in range(12):
        p.op("pe", lambda e, mc=mc: e.transpose(PS[7][:, 0:5], craw[0:5, mc * 128:(mc + 1) * 128], C.ident_f[0:5, 0:5]),
             reads=[craw_t, C.t], writes=[PS_t[7]])
        p.op("dve", lambda e, mc=mc: e.tensor_copy(cwT[:, mc, 0:5], PS[7][:, 0:5]), reads=[PS_t[7]], writes=[cwT_t])
    wt = [p.sbuf("ewt%d" % i, [128, 8, 256], BF16) for i in range(2)]
    wt_t = [T(), T()]
    xc = [p.sbuf("xc%d" % i, [128, 32 + NTOK], F32) for i in range(2)]
    xc_t = [T(), T()]
    cv = [p.sbuf("cv%d" % i, [128, NTOK], F32) for i in range(2)]
    cv_t = [T(), T()]
    xo = [p.sbuf("xo%d" % i, [128, NTOK], BF16) for i in range(2)]
    xo_t = [T(), T()]
    for mp in range(6):
        w, w_t = wt[mp % 2], wt_t[mp % 2]
        p.dma("pool", lambda e, w=w, mp=mp: e.dma_start(out=w[:], in_=winv[:, :, COL_XBC + mp * 256:COL_XBC + (mp + 1) * 256]),
              reads=[win_dt], writes=[w_t])
        for q in range(2):
            mc = mp * 2 + q
            x_, x_t = xc[mc % 2], xc_t[mc % 2]
            for ti, (c0, n) in enumerate(TT):
                ps, ps_t = PS[ti % 4], PS_t[ti % 4]
                for k in range(8):
                    p.op("pe", lambda e, ps=ps, w=w, k=k, q=q, c0=c0, n=n: e.matmul(
                        ps[:, 0:n], w[:, k, q * 128:(q + 1) * 128], hT[:, k, c0:c0 + n], start=(k == 0), stop=(k == 7)),
                        reads=[w_t, hT_t], writes=[ps_t])
                p.op("act", lambda e, x_=x_, ps=ps, c0=c0, n=n: e.copy(x_[:, c0:c0 + n], ps[:, 0:n]), reads=[ps_t], writes=[x_t])
            c_, c_t = cv[mc % 2], cv_t[mc % 2]
            for k in range(4):
                off = 32 - 3 + k
                if k == 0:
                    p.op("dve", lambda e, c_=c_, x_=x_, mc=mc, off=off: e.tensor_scalar(
                        c_[:], x_[:, off:off + NTOK], cwT[:, mc, 0:1], cwT[:, mc, 4:5], ALU.mult, ALU.add),
                        reads=[x_t, cwT_t], writes=[c_t])
                else:
                    p.op("dve", lambda e, c_=c_, x_=x_, mc=mc, off=off, k=k: e.scalar_tensor_tensor(
                        c_[:], x_[:, off:off + NTOK], cwT[:, mc, k:k + 1], c_[:], ALU.mult, ALU.add),
                        reads=[x_t, cwT_t, c_t], writes=[c_t])
            if mc >= 10:
                g = mc - 10
                p.op("act", lambda e, c_=c_, g=g: e.activation(CT[:, g, :], c_[:], ACTF.Silu), reads=[c_t], writes=[CT_t])
                continue
            if mc < 8:
                o_, o_t = xo[mc % 2], xo_t[mc % 2]
                p.op("act", lambda e, o_=o_, c_=c_: e.activation(o_[:], c_[:], ACTF.Silu), reads=[c_t], writes=[o_t])
                src_ap, src_t = o_[:], o_t
            else:
                g = mc - 8
                p.op("act", lambda e, c_=c_, g=g: e.activation(BT[:, g, :], c_[:], ACTF.Silu), reads=[c_t], writes=[BT_t])
                src_ap, src_t = BT[:, g, :], BT_t
            for ih in range(2):
                psb = PS[4 + ih][:].bitcast(BF16)
                for j in range(8):
                    i = ih * 8 + j
                    p.op("pe", lambda e, psb=psb, j=j, i=i, src_ap=src_ap: e.transpose(
                        psb[:, j * 128:(j + 1) * 128], src_ap[:, i * 128:(i + 1) * 128], C.ident_b[:]),
                        reads=[src_t, C.t], writes=[PS_t[4 + ih]])
                if mc < 8:
                    dst = xs[:, ih * 8:(ih + 1) * 8, mc * 128:(mc + 1) * 128]
                    dst_ts = xs_t[ih * 8:(ih + 1) * 8]
                else:
                    dst = btok[:, ih * 8:(ih + 1) * 8, (mc - 8) * 128:(mc - 7) * 128]
                    dst_ts = btok_t[ih * 8:(ih + 1) * 8]
                if ih == 0:
                    p.op("dve", lambda e, psb=psb, dst=dst: e.tensor_copy(dst, psb.rearrange("p (j c) -> p j c", j=8)),
                         reads=[PS_t[4 + ih]], writes=dst_ts)
                else:
                    p.op("act", lambda e, psb=psb, dst=dst: e.copy(dst, psb.rearrange("p (j c) -> p j c", j=8)),
                         reads=[PS_t[4 + ih]], writes=dst_ts)
    wdt = p.sbuf("wdt", [128, 8, NH], BF16); wdt_t = T()
    p.dma("pool", lambda e: e.dma_start(out=wdt[:], in_=winv[:, :, COL_DT:COL_DT + NH]), reads=[win_dt], writes=[wdt_t])
    for i in range(NCH):
        for k in range(8):
            p.op("pe", lambda e, i=i, k=k: e.matmul(PS[6][:, i * NH:(i + 1) * NH], hT[:, k, 32 + i * 128:32 + (i + 1) * 128],
                                                    wdt[:, k, :], start=(k == 0), stop=(k == 7)),
                 reads=[hT_t, wdt_t], writes=[PS_t[6]])
    sp_ = p.sbuf("sp_", [128, NCH, NH], F32)
    cum = p.sbuf("cum", [128, NCH, NH], F32)
    tot = p.sbuf("tot", [128, NCH, NH], F32)
    p.op("dve", lambda e: e.tensor_tensor(dt[:], PS[6][:, 0:NCH * NH].rearrange("p (i h) -> p i h", i=NCH),
                                          bc_mid(hp16[:, 0, :], NCH), ALU.add), reads=[PS_t[6], hp16_t], writes=[dd_t])
    p.op("act", lambda e: e.activation(sp_[:], dt[:], ACTF.Abs), reads=[dd_t], writes=[dd_t])
    p.op("act", lambda e: e.activation(sp_[:], sp_[:], ACTF.Exp, scale=-1.0), reads=[dd_t], writes=[dd_t])
    p.op("act", lambda e: e.activation(sp_[:], sp_[:], ACTF.Ln, bias=1.0), reads=[dd_t], writes=[dd_t])
    p.op("dve", lambda e: e.scalar_tensor_tensor(dt[:], dt[:], 0.0, sp_[:], ALU.max, ALU.add), reads=[dd_t], writes=[dd_t])
    p.op("dve", lambda e: e.tensor_tensor(da[:], dt[:], bc_mid(hp16[:, 1, :], NCH), ALU.mult), reads=[dd_t, hp16_t], writes=[dd_t])
    for i in range(NCH):
        p.op("pe", lambda e, i=i: e.matmul(PS[7][:, i * NH:(i + 1) * NH], U_f[:], da[:, i, :], start=True, stop=True),
             reads=[dd_t, cst_t], writes=[PS_t[7]])
        p.op("pe", lambda e, i=i: e.matmul(PS[7][:, 256 + i * NH:256 + (i + 1) * NH], ones_f[:], da[:, i, :], start=True, stop=True),
             reads=[dd_t, cst_t], writes=[PS_t[7]])
    p.op("dve", lambda e: e.tensor_copy(cum[:], PS[7][:, 0:256].rearrange("p (i h) -> p i h", i=NCH)), reads=[PS_t[7]], writes=[dd_t])
    p.op("dve", lambda e: e.tensor_copy(tot[:], PS[7][:, 256:512].rearrange("p (i h) -> p i h", i=NCH)), reads=[PS_t[7]], writes=[dd_t])
    p.op("dve", lambda e: e.reduce_sum(logD[:], tot[:].rearrange("p i h -> p h i"), AX.X), reads=[dd_t], writes=[logD_t])
    p.op("act", lambda e: e.activation(Ea[:], cum[:], ACTF.Exp), reads=[dd_t], writes=[dd_t])
    p.op("act", lambda e: e.activation(dec[:], tot[:], ACTF.Exp), reads=[dd_t], writes=[dd_t])
    p.op("dve", lambda e: e.tensor_tensor(wst[:], tot[:], cum[:], ALU.subtract), reads=[dd_t], writes=[dd_t])
    p.op("act", lambda e: e.activation(wst[:], wst[:], ACTF.Exp), reads=[dd_t], writes=[dd_t])
    p.op("dve", lambda e: e.tensor_tensor(wst[:], wst[:], dt[:], ALU.mult), reads=[dd_t], writes=[dd_t])
    for i in range(NCH):
        state_step(i)
    p.dma("sp", lambda e: e.dma_start(out=st_src[:, 0:D], in_=state[:]), reads=[state_t], writes=[st_src_t])
    p.dma("sp", lambda e: e.dma_start(out=st_src[:, D:D + NH], in_=logD[:]), reads=[logD_t], writes=[st_src_t])
    p.end_phase()
    p.end_phase()
    emit_allgather(p, st_src, st_src_t, st_gat, st_gat_t)


    p.begin_phase()
    lall = p.sbuf("lall", [128, 8, NH], F32); lall_t = T()
    p.dma("sp", lambda e: e.dma_start(out=lall[:], in_=st_gat.rearrange("(r p) c -> p r c", p=128)[:, :, D:D + NH]),
          reads=[st_gat_t], writes=[lall_t])
    bef = p.sbuf("bef", [128, 8], F32)
    btw = p.sbuf("btw", [128, 64], F32)
    p.dma("sp", lambda e: e.dma_start(out=bef[:], in_=before_d.partition_broadcast(128)), writes=[lall_t])
    p.dma("sp", lambda e: e.dma_start(out=btw[:], in_=between_d.partition_broadcast(128)), writes=[lall_t])
    coef = p.sbuf("coef", [128, 8, NH], F32); coef_t = T()
    p.op("pool", lambda e: e.memset(coef[:], 0.0), writes=[coef_t])
    for r in range(8):
        for r2 in range(8):
            p.op("dve", lambda e, r=r, r2=r2: e.scalar_tensor_tensor(coef[:, r, :], lall[:, r2, :], btw[:, r * 8 + r2:r * 8 + r2 + 1],
                                                                   coef[:, r, :], ALU.mult, ALU.add), reads=[lall_t, coef_t], writes=[coef_t])
    p.op("act", lambda e: e.activation(coef[:], coef[:], ACTF.Exp), reads=[coef_t], writes=[coef_t])
    p.op("dve", lambda e: e.tensor_tensor(coef[:], coef[:], bc_last(bef[:, :], NH), ALU.mult), reads=[coef_t, lall_t], writes=[coef_t])
    p.op("pool", lambda e: e.memset(state[:], 0.0), reads=[state_t], writes=[state_t])
    sr = [p.sbuf("sr%d" % i, [128, D], F32) for i in range(2)]
    sr_t = [T(), T()]
    for r in range(8):
        s_, s_t = sr[r % 2], sr_t[r % 2]
        p.dma("sp", lambda e, s_=s_, r=r: e.dma_start(out=s_[:], in_=st_gat[r * 128:(r + 1) * 128, 0:D]), reads=[st_gat_t], writes=[s_t])
        p.op("dve", lambda e, s_=s_, r=r: e.tensor_tensor(s_[:].rearrange("p (h q) -> p h q", h=NH), s_[:].rearrange("p (h q) -> p h q", h=NH),
                                                        bc_last(coef[:, r, :], HP), ALU.mult), reads=[s_t, coef_t], writes=[s_t])
        p.op("dve", lambda e, s_=s_: e.tensor_tensor(state[:], state[:], s_[:], ALU.add), reads=[s_t, state_t], writes=[state_t])
    p.op("act", lambda e: e.copy(state_b[:], state[:]), reads=[state_t], writes=[state_bt])
    p.end_phase()

    p.begin_phase()
    wo2 = p.sbuf("wo2", [128, 8, D], BF16); wo2_t = T()
    for half in range(2):
        p.dma("pool", lambda e, half=half: e.dma_start(out=wo2[:, :, half * 512:(half + 1) * 512],
                                                      in_=woutv[:, 8:16, half * 512:(half + 1) * 512]), reads=[wout_dt], writes=[wo2_t])
    DB = p.sbuf("DB", [128, D], F32)
    nwB = p.sbuf("nwB", [128, D], F32)
    mgB = p.sbuf("emgB", [128, D], F32)
    mbB = p.sbuf("embB", [128, D], F32)
    gb_t = T()
    p.op("pool", lambda e: e.memset(DB[:], 1.0), writes=[gb_t])
    p.op("dve", lambda e: e.tensor_tensor(DB[:].rearrange("p (h q) -> p h q", h=NH), DB[:].rearrange("p (h q) -> p h q", h=NH),
                                          bc_last(hp16[:, 2, :], HP), ALU.mult), reads=[gb_t, hp16_t], writes=[gb_t])
    p.dma("sp", lambda e: e.dma_start(out=nwB[:], in_=nw_d.partition_broadcast(128)), writes=[gb_t])
    p.dma("sp", lambda e: e.dma_start(out=mgB[:], in_=mg_d.partition_broadcast(128)), writes=[gb_t])
    p.dma("sp", lambda e: e.dma_start(out=mbB[:], in_=mb_d.partition_broadcast(128)), writes=[gb_t])
    xd = [p.sbuf("xd%d" % i, [128, D], BF16) for i in range(2)]; xd_t = [T(), T()]
    cbm = [p.sbuf("cbm%d" % i, [128, 2, 128], F32) for i in range(2)]; cbm_t = [T(), T()]
    lh = [p.sbuf("lh%d" % i, [128, 128], F32) for i in range(4)]; lh_t = [T() for _ in range(4)]
    Eb = [p.sbuf("Eb%d" % i, [128, 4, 128], F32) for i in range(2)]; Eb_t = [T(), T()]
    MT = [p.sbuf("MT%d" % i, [128, 4, 128], BF16) for i in range(2)]; MT_t = [T(), T()]
    yb = [p.sbuf("yb%d" % i, [128, D], F32) for i in range(2)]; yb_t = [T(), T()]
    t2 = [p.sbuf("t2_%d" % i, [128, D], F32) for i in range(2)]; t2_t = [T(), T()]
    ybf = [p.sbuf("ybf%d" % i, [128, D], BF16) for i in range(2)]; ybf_t = [T(), T()]
    yT = [p.sbuf("yT%d" % i, [128, 8, 128], BF16) for i in range(2)]; yT_t = [T(), T()]
    rr = [p.sbuf("rr%d" % i, [128, 4], F32) for i in range(2)]; rr_t = [T(), T()]
    hx = [p.sbuf("e2hx%d" % i, [128, D], F32) for i in range(2)]; hx_t = [T(), T()]
    lst = [p.sbuf("elst%d" % i, [128, 16], F32) for i in range(2)]; lst_t = [T(), T()]
    nlh = 0
    neb = 0
    for i in range(NCH):
        b = i % 2
        xd_, xdt = xd[b], xd_t[b]
        p.op("dve", lambda e, xd_=xd_, i=i: e.tensor_tensor(xd_[:].rearrange("p (h q) -> p h q", h=NH), xs[:, i, :].rearrange("p (h q) -> p h q", h=NH),
                                                          bc_last(dt[:, i, :], HP), ALU.mult), reads=[xs_t[i], dd_t], writes=[xdt])
        cb_, cbt = cbm[b], cbm_t[b]
        y_, yt = yb[b], yb_t[b]
        for g in range(2):
            p.op("pe", lambda e, g=g, i=i: e.matmul(PS[0][:, g * 128:(g + 1) * 128], BT[:, g, i * 128:(i + 1) * 128],
                                                    CT[:, g, i * 128:(i + 1) * 128], start=True, stop=True),
                 reads=[BT_t, CT_t], writes=[PS_t[0]])
            p.op("dve", lambda e, g=g, cb_=cb_: e.tensor_tensor(cb_[:, g, :], PS[0][:, g * 128:(g + 1) * 128], U_f[:], ALU.mult),
                 reads=[PS_t[0], cst_t], writes=[cbt])
            for hq in range(2):
                for h4 in range(4):
                    h = g * 8 + hq * 4 + h4
                    l_, l_t = lh[nlh % 4], lh_t[nlh % 4]
                    nlh += 1
                    p.op("dve", lambda e, l_=l_, i=i, h=h: e.tensor_scalar(l_[:], Ls_f[:], da[:, i, h:h + 1], None, ALU.mult),
                         reads=[cst_t, dd_t], writes=[l_t])
                    p.op("pe", lambda e, l_=l_, hq=hq, h4=h4: e.matmul(PS[1 + hq][:, h4 * 128:(h4 + 1) * 128], l_[:], U_f[:], start=True, stop=True),
                         reads=[l_t, cst_t], writes=[PS_t[1 + hq]])
                E_, E_t = Eb[neb % 2], Eb_t[neb % 2]
                M_, M_t = MT[neb % 2], MT_t[neb % 2]
                neb += 1
                p.op("act", lambda e, E_=E_, hq=hq: e.activation(E_[:], PS[1 + hq][:].rearrange("p (a s) -> p a s", a=4), ACTF.Exp),
                     reads=[PS_t[1 + hq]], writes=[E_t])
                p.op("pool", lambda e, E_=E_, M_=M_, cb_=cb_, g=g: e.tensor_tensor(M_[:], E_[:], bc_mid(cb_[:, g, :], 4), ALU.mult),
                     reads=[E_t, cbt], writes=[M_t])
                for h4 in range(4):
                    h = g * 8 + hq * 4 + h4
                    hh = hq * 4 + h4
                    p.op("pe", lambda e, M_=M_, h4=h4, h=h, hh=hh, g=g, xd_=xd_: e.matmul(PS[3 + g][:, hh * HP:(hh + 1) * HP], M_[:, h4, :],
                                                                                      xd_[:, h * HP:(h + 1) * HP], start=True, stop=True),
                         reads=[M_t, xdt], writes=[PS_t[3 + g]])
            p.op("pe", lambda e, g=g, i=i: e.matmul(PS[5 + g][:], CT[:, g, i * 128:(i + 1) * 128], state_b[:, g * 512:(g + 1) * 512],
                                                    start=True, stop=True), reads=[CT_t, state_bt], writes=[PS_t[5 + g]])
            p.op("dve", lambda e, g=g, i=i, y_=y_: e.tensor_tensor(y_[:, g * 512:(g + 1) * 512].rearrange("p (h q) -> p h q", h=8),
                                                                 PS[5 + g][:].rearrange("p (h q) -> p h q", h=8),
                                                                 bc_last(Ea[:, i, g * 8:(g + 1) * 8], HP), ALU.mult),
                 reads=[PS_t[5 + g], dd_t], writes=[yt])
            p.op("dve", lambda e, g=g, y_=y_: e.tensor_tensor(y_[:, g * 512:(g + 1) * 512], y_[:, g * 512:(g + 1) * 512], PS[3 + g][:], ALU.add),
                 reads=[PS_t[3 + g], yt], writes=[yt])
        t_, tt_ = t2[b], t2_t[b]
        p.op("pool", lambda e, t_=t_, i=i: e.tensor_tensor(t_[:], xs[:, i, :], DB[:], ALU.mult), reads=[xs_t[i], gb_t], writes=[tt_])
        p.op("pool", lambda e, t_=t_, y_=y_: e.tensor_tensor(y_[:], y_[:], t_[:], ALU.add), reads=[tt_, yt], writes=[yt])
        p.op("pool", lambda e, y_=y_, i=i: e.tensor_tensor(y_[:], y_[:], sz[:, i, :], ALU.mult), reads=[yt, sz_t[i]], writes=[yt])
        r_, r_t = rr[b], rr_t[b]
        p.op("act", lambda e, t_=t_, y_=y_: e.activation(t_[:], y_[:], ACTF.Square), reads=[yt, tt_], writes=[tt_])
        p.op("dve", lambda e, r_=r_, t_=t_: e.reduce_sum(r_[:, 0:2], t_[:].rearrange("p (g c) -> p g c", g=2), AX.X), reads=[tt_], writes=[r_t])
        p.op("act", lambda e, r_=r_: e.activation(r_[:, 0:2], r_[:, 0:2], ACTF.Sqrt, bias=LN_EPS, scale=1.0 / 512), reads=[r_t], writes=[r_t])
        p.op("dve", lambda e, r_=r_: e.reciprocal(r_[:, 0:2], r_[:, 0:2]), reads=[r_t], writes=[r_t])
        yf, yft = ybf[b], ybf_t[b]
        for g in range(2):
            p.op("dve", lambda e, g=g, yf=yf, y_=y_, r_=r_: e.scalar_tensor_tensor(yf[:, g * 512:(g + 1) * 512], y_[:, g * 512:(g + 1) * 512],
                                                                                 r_[:, g:g + 1], nwB[:, g * 512:(g + 1) * 512], ALU.mult, ALU.mult),
                 reads=[yt, r_t, gb_t], writes=[yft])
        psb = PS[7][:].bitcast(BF16)
        for k in range(8):
            p.op("pe", lambda e, k=k, yf=yf, psb=psb: e.transpose(psb[:, k * 128:(k + 1) * 128], yf[:, k * 128:(k + 1) * 128], C.ident_b[:]),
                 reads=[yft, C.t], writes=[PS_t[7]])
        yT_, yTt = yT[b], yT_t[b]
        p.op("act", lambda e, yT_=yT_, psb=psb: e.copy(yT_[:], psb.rearrange("p (k t) -> p k t", k=8)), reads=[PS_t[7]], writes=[yTt])
        x, x_t = hx[b], hx_t[b]
        p.dma("sp", lambda e, x=x, i=i: e.dma_start(out=x[:], in_=hacc_d[i * 128:(i + 1) * 128, :]), reads=[hacc_dt], writes=[x_t])
        for dh in range(2):
            for k in range(8):
                p.op("pe", lambda e, k=k, dh=dh, yT_=yT_: e.matmul(PS[1 + dh][:], yT_[:, k, :], wo2[:, k, dh * 512:(dh + 1) * 512],
                                                                 start=(k == 0), stop=(k == 7)), reads=[yTt, wo2_t], writes=[PS_t[1 + dh]])
            p.op("dve", lambda e, x=x, dh=dh: e.tensor_tensor(x[:, dh * 512:(dh + 1) * 512], x[:, dh * 512:(dh + 1) * 512], PS[1 + dh][:], ALU.add),
                 reads=[PS_t[1 + dh], x_t], writes=[x_t])
        emit_ln(p, x[:], x_t, mgB, mbB, gb_t, lst[b], lst_t[b], eng2="pool")
        p.dma("sp", lambda e, x=x, i=i: e.dma_start(out=h1_d[i * 128:(i + 1) * 128, :], in_=x[:]), reads=[x_t], writes=[h1_dt])
        state_step(i)
    p.end_phase()
    p.end_phase()


def core_consts(c):
    b, pos = c // 4, c % 4
    sel = np.zeros((256, 32), np.float32)
    if pos != 0:
        for j in range(32):
            sel[(c - 1) * 32 + j, j] = 1.0
    before = np.zeros((8,), np.float32)
    between = np.zeros((8, 8), np.float32)
    for r in range(8):
        if r // 4 == b and r < c:
            before[r] = 1.0
            for r2 in range(8):
                if r2 // 4 == b and r < r2 < c:
                    between[r, r2] = 1.0
    hasprev = np.array([1.0 if pos != 0 else 0.0], np.float32)
    return dict(sel=sel, before=before, between=between.reshape(64), hasprev=hasprev)


def build_even_only():
    nc = bass.Bass("TRN2", target_bir_lowering=False)
    def inp(name, shape):
        return nc.dram_tensor(name, list(shape), F32, kind="ExternalInput").ap()
    h = inp("h", [NTOK, D]); sel = inp("sel", [256, 32]); before = inp("before", [8]); between = inp("between", [64])
    win = inp("win", [D, 5648]); wout = inp("wout", [2 * D, D])
    scw = inp("scw", [3, D]); cw = inp("cw", [4, 1536]); cb = inp("cb", [1536])
    dtb = inp("dtb", [NH]); alog = inp("alog", [NH]); dsk = inp("dsk", [NH]); nw = inp("nw", [D])
    mg = inp("mg", [D]); mb = inp("mb", [D])
    h1 = nc.dram_tensor("h1", [NTOK, D], F32, kind="ExternalOutput").ap()
    halo = nc.dram_tensor("halo_i", [32, D], F32).ap()
    p = Prog(nc)
    C = Consts(p)
    halo_t = T()
    emit_halo(p, C, nc, h, T(), sel, halo, halo_t, "t0")
    emit_even(p, C, nc, "t0", h, T(), halo, halo_t, h1, T(), win, T(), wout, T(), scw, cw, cb, dtb, alog, dsk, nw, mg, mb, before, between)
    p.finish()
    return nc


SMALL = [("sc_conv_w", [2, 3, D]), ("ssm_conv_w", [2, 4, 1536]), ("ssm_conv_b", [2, 1536]), ("ssm_dt_bias", [2, NH]),
         ("ssm_a_log", [2, NH]), ("ssm_d", [2, NH]), ("ssm_norm_w", [2, D]), ("conf_b_pw1", [2, 2 * D]),
         ("conf_w_dw", [2, CONF_K, D]), ("conf_b_dw", [2, D]), ("conf_ln_g", [2, D]), ("conf_ln_b", [2, D]),
         ("conf_b_pw2", [2, D]), ("router_w", [DEPTH, D, NE]), ("router_b", [DEPTH, NE]), ("exp_b_gu", [DEPTH, NE, 2 * D]),
         ("exp_b_down", [DEPTH, NE, D]), ("ln_mix_g", [DEPTH, D]), ("ln_mix_b", [DEPTH, D]), ("ln_ffn_g", [DEPTH, D]),
         ("ln_ffn_b", [DEPTH, D])]


def build_full(nl=DEPTH):
    nc = bass.Bass("TRN2", target_bir_lowering=False)

    def inp(name, shape):
        return nc.dram_tensor(name, list(shape), F32, kind="ExternalInput").ap()

    def internal(name, shape):
        return nc.dram_tensor(name, list(shape), F32).ap()

    x = inp("x", [NTOK, D])
    halo0 = inp("halo0", [32, D])
    sel = inp("sel", [256, 32]); before = inp("before", [8]); between = inp("between", [64]); hasprev = inp("hasprev", [1])
    win = inp("mix_w_in", [2, D, 5648]); wout = inp("mix_w_out", [2, 2 * D, D])
    pw1 = inp("conf_w_pw1", [2, D, 2 * D]); pw2 = inp("conf_w_pw2", [2, D, D])
    wgu = inp("exp_w_gu", [DEPTH, NE, D, 2 * D]); wd = inp("exp_w_down", [DEPTH, NE, D, D])
    sm = {name: inp(name, shape) for name, shape in SMALL}
    out = nc.dram_tensor("out", [NTOK, D], F32, kind="ExternalOutput").ap()

    p = Prog(nc)
    C = Consts(p)
    wt_ = T()
    h_cur, h_cur_t = x, T()
    for l in range(nl):
        j = l // 2
        tag = "L%d" % l
        if l == 0:
            halo, halo_t = halo0, T()
        else:
            halo = internal("halo_" + tag, [32, D]); halo_t = T()
            emit_halo(p, C, nc, h_cur, h_cur_t, sel, halo, halo_t, tag)
        hmid = internal("hmid_" + tag, [NTOK, D]); hmid_t = T()
        if l % 2 == 0:
            emit_even(p, C, nc, tag, h_cur, h_cur_t, halo, halo_t, hmid, hmid_t, win[j], wt_, wout[j], wt_,
                      sm["sc_conv_w"][j], sm["ssm_conv_w"][j], sm["ssm_conv_b"][j], sm["ssm_dt_bias"][j], sm["ssm_a_log"][j],
                      sm["ssm_d"][j], sm["ssm_norm_w"][j], sm["ln_mix_g"][l], sm["ln_mix_b"][l], before, between)
        else:
            emit_conformer(p, C, h_cur, h_cur_t, halo, halo_t, hasprev, hmid, hmid_t, pw1[j], wt_, sm["conf_b_pw1"][j],
                           sm["conf_w_dw"][j], sm["conf_b_dw"][j], sm["conf_ln_g"][j], sm["conf_ln_b"][j], pw2[j], wt_,
                           sm["conf_b_pw2"][j], sm["ln_mix_g"][l], sm["ln_mix_b"][l])
        if l == nl - 1:
            h_next, h_next_t = out, T()
        else:
            h_next, h_next_t = internal("h_" + tag, [NTOK, D]), T()
        emit_moe(p, C, hmid, hmid_t, h_next, h_next_t, sm["router_w"][l], sm["router_b"][l],
                 lambda ex, l=l: (wgu[l, ex], wt_), sm["exp_b_gu"][l], lambda ex, l=l: (wd[l, ex], wt_), sm["exp_b_down"][l],
                 sm["ln_ffn_g"][l], sm["ln_ffn_b"][l])
        h_cur, h_cur_t = h_next, h_next_t
    p.finish()
    return nc


_NC_CACHE = {}
BIG = ["mix_w_in", "mix_w_out", "conf_w_pw1", "conf_w_pw2", "exp_w_gu", "exp_w_down"]


def kernel(_nl=DEPTH, **inputs):
    f = np.float32
    A = lambda k: np.ascontiguousarray(np.asarray(inputs[k], dtype=f))
    x = A("x").reshape(8, NTOK, D)
    shared = {name: A(name) for name, _ in SMALL}
    for k in BIG:
        shared[k] = A(k)
    zeros = np.zeros((32, D), f)
    in_maps = []
    for c in range(8):
        cc = core_consts(c)
        halo0 = x[c - 1, NTOK - 32:, :] if c % 4 != 0 else zeros
        m = dict(x=x[c], halo0=np.ascontiguousarray(halo0), sel=cc["sel"], before=cc["before"], between=cc["between"],
                 hasprev=cc["hasprev"])
        m.update(shared)
        in_maps.append(m)
    if _nl not in _NC_CACHE:
        _NC_CACHE[_nl] = build_full(_nl)
    res = run_bass_kernel_spmd(_NC_CACHE[_nl], in_maps, core_ids=list(range(8)))
    outs = [np.asarray(res.results[c]["out"], dtype=f) for c in range(8)]
    return np.stack(outs).reshape(2, 4 * NTOK, D)
```

```python
from contextlib import ExitStack
import numpy as np
import concourse.bass as bass
import concourse.mybir as mybir
from concourse.bass_utils import run_bass_kernel_spmd

F32 = mybir.dt.float32
BF16 = mybir.dt.bfloat16
ALU = mybir.AluOpType
ACTF = mybir.ActivationFunctionType
AX = mybir.AxisListType

D = 1024
NTOK = 2048
NCH = NTOK // 128
NE = 32
CAP = 384
NJC = CAP // 128
DEPTH = 4
ALPHA = (2 * DEPTH) ** 0.25
LN_EPS = 1e-5
SW_LIMIT = 7.0
SW_ALPHA = 1.702

EP = 30000
NS = 16
ENGS = ("pe", "dve", "act", "pool", "sp")


class T:
    __slots__ = ("name", "w", "r")

    def __init__(self, name=""):
        self.name = name
        self.w = None
        self.r = {}


class Prog:
    def __init__(self, nc):
        self.nc = nc
        self.es = ExitStack()
        self.phs = []
        self.ops = {e: [] for e in ENGS}
        self.ccount = {e: 0 for e in ENGS}
        self.dcount = {e: 0 for e in ENGS}
        self.known = {e: {} for e in ENGS}
        self.sems = {}
        self.nsem = 0
        self.nname = 0
        self.xcount = 0

    def _nm(self, name):
        self.nname += 1
        return "%s_%d" % (name, self.nname)

    def sbuf(self, name, shape, dt, persistent=False):
        st = self.es if (persistent or not self.phs) else self.phs[-1]
        return st.enter_context(self.nc.sbuf_tensor(self._nm(name), list(shape), dt))

    def psum(self, name, shape, dt, persistent=False):
        st = self.es if (persistent or not self.phs) else self.phs[-1]
        return st.enter_context(self.nc.psum_tensor(self._nm(name), list(shape), dt))

    def begin_phase(self):
        self.phs.append(ExitStack())

    def end_phase(self):
        self.barrier()
        self.flush()
        self.phs.pop().close()

    def _sem(self, key):
        s = self.sems.get(key)
        if s is None:
            self.nsem += 1
            s = self.es.enter_context(self.nc.semaphore("s%d" % self.nsem))
            self.sems[key] = s
        return s

    def _need(self, eng, tok, waits):
        if tok is None:
            return
        key, val = tok
        if key[0] == "c" and key[1] == eng and eng == "pe":
            return
        if self.known[eng].get(key, 0) >= val:
            return
        self.known[eng][key] = val
        waits.append(tok)

    def _deps(self, eng, reads, writes):
        waits = []
        for t in reads:
            self._need(eng, t.w, waits)
        for t in writes:
            self._need(eng, t.w, waits)
            for k, v in t.r.items():
                self._need(eng, (k, v), waits)
        return waits

    def _mark(self, tok, reads, writes):
        key, val = tok
        ws = set(id(t) for t in writes)
        for t in writes:
            t.w = tok
            t.r = {}
        for t in reads:
            if id(t) in ws:
                continue
            if t.r.get(key, 0) < val:
                t.r[key] = val

    def op(self, eng, fn, reads=(), writes=()):
        waits = self._deps(eng, reads, writes)
        self.ccount[eng] += 1
        tok = (("c", eng), self.ccount[eng])
        self.ops[eng].append((waits, fn, tok))
        self._mark(tok, reads, writes)
        return tok

    def dma(self, eng, fn, reads=(), writes=()):
        waits = self._deps(eng, reads, writes)
        n = self.dcount[eng]
        self.dcount[eng] += 1
        slot = n % NS
        key = ("d", eng, slot)
        if n >= NS:
            self._need(eng, (key, 16 * (n // NS)), waits)
        tok = (key, 16 * (n // NS + 1))
        self.ops[eng].append((waits, fn, tok))
        self._mark(tok, reads, writes)
        return tok

    def coll(self, fn, reads=(), writes=()):
        waits = self._deps("pool", reads, writes)
        self.xcount += 1
        tok = (("x",), self.xcount)
        self.ops["pool"].append((waits, fn, tok))
        self._mark(tok, reads, writes)
        return tok

    def _all_tokens(self):
        toks = [(("x",), self.xcount)] if self.xcount else []
        for e in ENGS:
            if self.ccount[e]:
                toks.append((("c", e), self.ccount[e]))
            n = self.dcount[e]
            for slot in range(min(n, NS)):
                last = ((n - 1 - slot) // NS) * NS + slot
                toks.append((("d", e, slot), 16 * (last // NS + 1)))
        return toks

    def barrier(self, engs=ENGS):
        toks = self._all_tokens()
        for e in engs:
            waits = []
            for tok in toks:
                self._need(e, tok, waits)
            if waits:
                self.ops[e].append((waits, None, None))

    def _semval(self, tok):
        key, val = tok
        if key[0] == "c":
            ep = (val - 1) // EP
            return self._sem(("c", key[1], ep)), (val - 1) % EP + 1
        return self._sem(key), val

    def flush(self):
        nc = self.nc
        engobj = {"pe": "tensor", "dve": "vector", "act": "scalar", "pool": "gpsimd", "sp": "sync"}
        for e in ENGS:
            for waits, fn, tok in self.ops[e]:
                for w in waits:
                    self._semval(w)
                if tok is not None:
                    self._semval(tok)
        if not any(self.ops[e] for e in ENGS):
            return
        with nc.Block() as block:
            for e in ENGS:
                if not self.ops[e]:
                    continue

                def body(engine, e=e):
                    for waits, fn, tok in self.ops[e]:
                        for w in waits:
                            s, v = self._semval(w)
                            engine.wait_ge(s, v)
                        if fn is None:
                            continue
                        ins = fn(engine)
                        s, v = self._semval(tok)
                        ins.then_inc(s, 16 if tok[0][0] == "d" else 1)

                getattr(block, engobj[e])(body)
        self.ops = {e: [] for e in ENGS}

    def finish(self):
        self.barrier()
        self.flush()
        self.es.close()


class Consts:
    def __init__(self, p):
        self.t = T("consts")
        self.ident_f = p.sbuf("ident_f", [128, 128], F32, persistent=True)
        self.ident_b = p.sbuf("ident_b", [128, 128], BF16, persistent=True)
        self.lstrict_b = p.sbuf("lstrict_b", [128, 128], BF16, persistent=True)
        self.ones_b = p.sbuf("ones_b", [128, 128], BF16, persistent=True)
        self.iota_c = p.sbuf("iota_c", [128, CAP], F32, persistent=True)
        c = self
        t = self.t
        p.op("pool", lambda e: e.memset(c.ident_f[:], 0.0), writes=[t])
        p.op("pool", lambda e: e.affine_select(c.ident_f[:], c.ident_f[:], [[-1, 128]], ALU.not_equal, 1.0,
                                               base=0, channel_multiplier=1), reads=[t], writes=[t])
        p.op("pool", lambda e: e.tensor_copy(c.ident_b[:], c.ident_f[:]), reads=[t], writes=[t])
        p.op("pool", lambda e: e.memset(c.ones_b[:], 1.0), writes=[t])
        p.op("pool", lambda e: e.memset(c.lstrict_b[:], 1.0), writes=[t])
        p.op("pool", lambda e: e.affine_select(c.lstrict_b[:], c.lstrict_b[:], [[1, 128]], ALU.is_gt, 0.0,
                                               base=0, channel_multiplier=-1), reads=[t], writes=[t])
        p.op("pool", lambda e: e.iota(c.iota_c[:], [[1, CAP]], base=0, channel_multiplier=0,
                                      allow_small_or_imprecise_dtypes=True), writes=[t])


def bcast_rows(ap_1d, nparts=128):
    return ap_1d.partition_broadcast(nparts)


def emit_ln(p, x_ap, x_t, g_tile, b_tile, gb_t, st, st_t, out_ap=None, out_t=None, eng2="pool"):
    if out_ap is None:
        out_ap, out_t = x_ap, x_t
    stats = st[:, 0:12]
    mv = st[:, 12:14]
    rstd = st[:, 14:15]
    p.op("dve", lambda e: e.bn_stats(st[:, 0:6], x_ap[:, 0:512]), reads=[x_t], writes=[st_t])
    p.op("dve", lambda e: e.bn_stats(st[:, 6:12], x_ap[:, 512:1024]), reads=[x_t, st_t], writes=[st_t])
    p.op("dve", lambda e: e.bn_aggr(mv, stats), reads=[st_t], writes=[st_t])
    p.op("act", lambda e: e.activation(rstd, st[:, 13:14], ACTF.Sqrt, bias=LN_EPS), reads=[st_t], writes=[st_t])
    p.op("dve", lambda e: e.reciprocal(rstd, rstd), reads=[st_t], writes=[st_t])
    p.op("dve", lambda e: e.tensor_scalar(x_ap, x_ap, st[:, 12:13], rstd, ALU.subtract, ALU.mult),
         reads=[x_t, st_t], writes=[x_t])
    p.op(eng2, lambda e: e.tensor_tensor(x_ap, x_ap, g_tile[:], ALU.mult), reads=[x_t, gb_t], writes=[x_t])
    p.op(eng2, lambda e: e.tensor_tensor(out_ap, x_ap, b_tile[:], ALU.add), reads=[x_t, gb_t], writes=[out_t] if out_t is not x_t else [x_t])


def emit_moe(p, C, h1_d, h1_dt, h2_d, h2_dt, wr_d, br_d, wgu_of, bgu_d, wd_of, bd_d, lng_d, lnb_d, order=None):
    nc = p.nc
    p.begin_phase()
    acc = p.sbuf("acc", [128, NCH, D], F32)
    acc_t = [T("acc%d" % i) for i in range(NCH)]
    xtok = p.sbuf("xtok", [128, NCH, D], BF16)
    xtok_t = [T() for _ in range(NCH)]
    gates = p.sbuf("gates", [128, NCH, NE], F32)
    ghl = p.sbuf("ghl", [128, NCH, NE, 2], BF16)
    posm = p.sbuf("posm", [128, NCH, NE], F32)
    maskb = p.sbuf("maskb", [128, NCH, NE], BF16)
    rt_t = [T() for _ in range(NCH)]
    bguT = p.sbuf("bguT", [128, 16, NE], F32); bguT_t = T()
    PS = [p.psum("ps%d" % i, [128, 512], F32) for i in range(8)]
    PS_t = [T("ps%d" % i) for i in range(8)]
    p.begin_phase()
    wr = p.sbuf("wr", [128, 8, NE], F32); wr_t = T()
    brb = p.sbuf("brb", [128, NE], F32)
    bd = p.sbuf("bd", [NE, D], F32)
    bgu_raw = p.sbuf("bgu_raw", [NE, 2 * D], F32)

    p.dma("sp", lambda e: e.dma_start(out=wr[:], in_=wr_d.rearrange("(k p) n -> p k n", p=128)), writes=[wr_t])
    p.dma("sp", lambda e: e.dma_start(out=brb[:], in_=bcast_rows(br_d)), writes=[wr_t])
    p.dma("sp", lambda e: e.dma_start(out=bd[:], in_=bd_d), writes=[wr_t])
    p.dma("sp", lambda e: e.dma_start(out=bgu_raw[:], in_=bgu_d), writes=[wr_t])
    for c4 in range(4):
        ps = PS[7]
        for q in range(4):
            c = c4 * 4 + q
            p.op("pe", lambda e, c=c, q=q, ps=ps: e.transpose(ps[:, q * NE:(q + 1) * NE],
                                                             bgu_raw[:, c * 128:(c + 1) * 128], C.ident_f[0:NE, 0:NE]),
                 reads=[wr_t, C.t], writes=[PS_t[7]])
        p.op("dve", lambda e, c4=c4, ps=ps: e.tensor_copy(bguT[:, c4 * 4:(c4 + 1) * 4, :],
                                                          ps[:, 0:4 * NE].rearrange("p (q e) -> p q e", q=4)),
             reads=[PS_t[7]], writes=[bguT_t])

    xin = [p.sbuf("xin%d" % i, [128, D], F32) for i in range(2)]
    xin_t = [T() for _ in range(2)]
    xT = [p.sbuf("xT%d" % i, [128, 8, 128], F32) for i in range(2)]
    xT_t = [T() for _ in range(2)]
    rs = [p.sbuf("rs%d" % i, [128, 96], F32) for i in range(2)]
    rs_t = [T() for _ in range(2)]
    gT = [p.sbuf("gT%d" % i, [NE, 128], F32) for i in range(2)]
    gT_t = [T() for _ in range(2)]
    for i in range(NCH):
        b = i % 2
        x, x_t = xin[b], xin_t[b]
        p.dma("sp", lambda e, x=x, i=i: e.dma_start(out=x[:], in_=h1_d[i * 128:(i + 1) * 128, :]),
              reads=[h1_dt], writes=[x_t])
        p.op("act", lambda e, x=x, i=i: e.copy(xtok[:, i, :], x[:]), reads=[x_t], writes=[xtok_t[i]])
        for half in range(2):
            ps, ps_t = PS[half], PS_t[half]
            for q in range(4):
                dc = half * 4 + q
                p.op("pe", lambda e, x=x, dc=dc, q=q, ps=ps: e.transpose(ps[:, q * 128:(q + 1) * 128],
                                                                     x[:, dc * 128:(dc + 1) * 128], C.ident_f[:]),
                     reads=[x_t, C.t], writes=[ps_t])
            p.op("dve" if half == 0 else "act",
                 (lambda e, ps=ps, b=b, half=half: e.tensor_copy(
                     xT[b][:, half * 4:(half + 1) * 4, :], ps[:].rearrange("p (q t) -> p q t", q=4))) if half == 0 else
                 (lambda e, ps=ps, b=b, half=half: e.copy(
                     xT[b][:, half * 4:(half + 1) * 4, :], ps[:].rearrange("p (q t) -> p q t", q=4))),
                 reads=[ps_t], writes=[xT_t[b]])
        for kc in range(8):
            p.op("pe", lambda e, b=b, kc=kc: e.matmul(PS[2][:, 0:NE], xT[b][:, kc, :], wr[:, kc, :],
                                                      start=(kc == 0), stop=(kc == 7)),
                 reads=[xT_t[b], wr_t], writes=[PS_t[2]])
        r, r_t = rs[b], rs_t[b]
        lg = r[:, 0:32]
        top8 = r[:, 32:40]
        nm1 = r[:, 40:41]
        ssum = r[:, 41:42]
        ex = r[:, 48:80]
        p.op("dve", lambda e, lg=lg: e.tensor_tensor(lg, PS[2][:, 0:NE], brb[:], ALU.add),
             reads=[PS_t[2], wr_t], writes=[r_t])
        p.op("dve", lambda e, lg=lg, top8=top8: e.max(top8, lg), reads=[r_t], writes=[r_t])
        p.op("dve", lambda e, r=r: e.tensor_scalar(r[:, 40:41], r[:, 32:33], -1.0, None, ALU.mult),
             reads=[r_t], writes=[r_t])
        p.op("act", lambda e, r=r: e.activation(r[:, 48:80], r[:, 0:32], ACTF.Exp, bias=r[:, 40:41]),
             reads=[r_t], writes=[r_t])
        p.op("dve", lambda e, r=r, i=i: e.tensor_scalar(gates[:, i, :], r[:, 0:32], r[:, 35:36], None, ALU.is_ge),
             reads=[r_t], writes=[rt_t[i]])
        p.op("dve", lambda e, i=i: e.tensor_copy(maskb[:, i, :], gates[:, i, :]), reads=[rt_t[i]], writes=[rt_t[i]])
        for i2 in range(i + 1):
            lhs = C.lstrict_b if i2 == i else C.ones_b
            p.op("pe", lambda e, lhs=lhs, i2=i2, i=i: e.matmul(PS[3][:, 0:NE], lhs[:], maskb[:, i2, :],
                                                               start=(i2 == 0), stop=(i2 == i)),
                 reads=[rt_t[i2], C.t], writes=[PS_t[3]])
        p.op("dve", lambda e, i=i: e.scalar_tensor_tensor(posm[:, i, :], PS[3][:, 0:NE], 1.0, gates[:, i, :],
                                                          ALU.add, ALU.mult),
             reads=[PS_t[3], rt_t[i]], writes=[rt_t[i]])
        p.op("dve", lambda e, i=i: e.tensor_scalar(posm[:, i, :], posm[:, i, :], -1.0, None, ALU.add),
             reads=[rt_t[i]], writes=[rt_t[i]])
        p.op("dve", lambda e, r=r, i=i: e.tensor_tensor(r[:, 48:80], r[:, 48:80], gates[:, i, :], ALU.mult),
             reads=[r_t, rt_t[i]], writes=[r_t])
        p.op("dve", lambda e, r=r: e.reduce_sum(r[:, 41:42], r[:, 48:80], AX.X), reads=[r_t], writes=[r_t])
        p.op("dve", lambda e, r=r: e.reciprocal(r[:, 41:42], r[:, 41:42]), reads=[r_t], writes=[r_t])
        p.op("dve", lambda e, r=r, i=i: e.tensor_scalar(gates[:, i, :], r[:, 48:80], r[:, 41:42], None, ALU.mult),
             reads=[r_t], writes=[rt_t[i]])
        p.op("dve", lambda e, i=i: e.tensor_copy(ghl[:, i, :, 0], gates[:, i, :]), reads=[rt_t[i]], writes=[rt_t[i]])
        p.op("dve", lambda e, i=i, r=r: e.tensor_tensor(r[:, 48:80], gates[:, i, :], ghl[:, i, :, 0], ALU.subtract),
             reads=[rt_t[i], r_t], writes=[r_t])
        p.op("dve", lambda e, i=i, r=r: e.tensor_copy(ghl[:, i, :, 1], r[:, 48:80]), reads=[r_t], writes=[rt_t[i]])
        p.op("pe", lambda e, i=i: e.transpose(PS[6][0:NE, 0:128], gates[:, i, :], C.ident_f[:]),
             reads=[rt_t[i], C.t], writes=[PS_t[6]])
        p.op("act", lambda e, b=b: e.copy(gT[b][:], PS[6][0:NE, 0:128]), reads=[PS_t[6]], writes=[gT_t[b]])
        for dh in range(2):
            p.op("pe", lambda e, b=b, dh=dh: e.matmul(PS[4 + dh][:], gT[b][:], bd[:, dh * 512:(dh + 1) * 512],
                                                      start=True, stop=True),
                 reads=[gT_t[b], wr_t], writes=[PS_t[4 + dh]])
            p.op("dve", lambda e, x=x, i=i, dh=dh: e.scalar_tensor_tensor(
                acc[:, i, dh * 512:(dh + 1) * 512], x[:, dh * 512:(dh + 1) * 512], ALPHA, PS[4 + dh][:],
                ALU.mult, ALU.add), reads=[x_t, PS_t[4 + dh]], writes=[acc_t[i]])

    p.end_phase()
    p.begin_phase()
    S2 = [p.sbuf("S%d" % i, [128, NCH, CAP], BF16) for i in range(2)]
    S2_t = [[T() for _ in range(NCH)] for _ in range(2)]
    XgT = p.sbuf("XgT", [128, 8, CAP], BF16)
    XgT_t = [T() for _ in range(8)]
    actT = p.sbuf("actT", [128, 8, CAP], BF16)
    actT_t = [T() for _ in range(8)]
    Yg = p.sbuf("Yg", [128, NJC, D], BF16)
    Yg_t = [T() for _ in range(NJC)]
    gsl = p.sbuf("gsl", [128, NJC], F32); gsl_t = T()
    NWG = 3
    wg = [p.sbuf("wg%d" % i, [128, 8, 2, 256], BF16) for i in range(NWG)]
    wg_t = [T() for _ in range(NWG)]
    NWD = 2
    wdt = [p.sbuf("wd%d" % i, [128, 8, 512], BF16) for i in range(NWD)]
    wdt_t = [T() for _ in range(NWD)]
    NTMP = 3
    tg = [p.sbuf("tg%d" % i, [128, CAP], F32) for i in range(NTMP)]
    tsg = [p.sbuf("tsg%d" % i, [128, CAP], F32) for i in range(NTMP)]
    tu = [p.sbuf("tu%d" % i, [128, CAP], F32) for i in range(NTMP)]
    tmp_t = [T() for _ in range(NTMP)]
    NST = 4
    ST = [p.sbuf("ST%d" % i, [128, NJC, 128], BF16) for i in range(NST)]
    ST_t = [T() for _ in range(NST)]
    nwg = 0
    nwd = 0
    ntmp = 0
    nst = 0
    ndn = 0
    nsc = 0
    exlist = list(order if order is not None else range(NE))

    def build_S(n):
        ex_ = exlist[n]
        Sb, Sb_t = S2[n % 2], S2_t[n % 2]
        for i in range(NCH):
            p.op("dve", lambda e, i=i, ex_=ex_, Sb=Sb: e.tensor_scalar(Sb[:, i, :], C.iota_c[:], posm[:, i, ex_:ex_ + 1], None,
                                                                    ALU.is_equal),
                 reads=[rt_t[i], C.t], writes=[Sb_t[i]])

    build_S(0)
    for n_ex, ex in enumerate(exlist):
        S, S_t = S2[n_ex % 2], S2_t[n_ex % 2]
        for dc in range(8):
            ps, ps_t = PS[dc % 2], PS_t[dc % 2]
            for i in range(NCH):
                p.op("pe", lambda e, ps=ps, i=i, dc=dc, S=S: e.matmul(ps[:, 0:CAP], xtok[:, i, dc * 128:(dc + 1) * 128],
                                                                S[:, i, :], start=(i == 0), stop=(i == NCH - 1)),
                     reads=[xtok_t[i], S_t[i]], writes=[ps_t])
            p.op("act", lambda e, ps=ps, dc=dc: e.copy(XgT[:, dc, :], ps[:, 0:CAP]), reads=[ps_t], writes=[XgT_t[dc]])
        if n_ex + 1 < len(exlist):
            build_S(n_ex + 1)
        for jc in range(NJC):
            for i in range(NCH):
                p.op("pe", lambda e, i=i, jc=jc, ex=ex, S=S: e.matmul(PS[6][:, 2 * jc:2 * jc + 2],
                                                                 S[:, i, jc * 128:(jc + 1) * 128], ghl[:, i, ex, :],
                                                                 start=(i == 0), stop=(i == NCH - 1)),
                     reads=[S_t[i], rt_t[i]], writes=[PS_t[6]])
        p.op("dve", lambda e: e.reduce_sum(gsl[:], PS[6][:, 0:2 * NJC].rearrange("p (j two) -> p j two", two=2), AX.X),
             reads=[PS_t[6]], writes=[gsl_t])
        for fp in range(4):
            w, w_t = wg[nwg % NWG], wg_t[nwg % NWG]
            nwg += 1
            for two in range(2):
                wsrc, wsrc_t = wgu_of(ex)
                src = wsrc.rearrange("(k p) f -> p k f", p=128)[:, :, two * D + fp * 256:two * D + (fp + 1) * 256]
                p.dma("pool", lambda e, w=w, src=src, two=two: e.dma_start(out=w[:, :, two, :], in_=src), reads=[wsrc_t], writes=[w_t])
            for q in range(2):
                fc = fp * 2 + q
                pgi, pui = 2 + 2 * (fc % 2), 3 + 2 * (fc % 2)
                for kc in range(8):
                    p.op("pe", lambda e, w=w, kc=kc, q=q, pgi=pgi: e.matmul(PS[pgi][:, 0:CAP], w[:, kc, 0, q * 128:(q + 1) * 128],
                                                                        XgT[:, kc, :], start=(kc == 0), stop=(kc == 7)),
                         reads=[w_t, XgT_t[kc]], writes=[PS_t[pgi]])
                for kc in range(8):
                    p.op("pe", lambda e, w=w, kc=kc, q=q, pui=pui: e.matmul(PS[pui][:, 0:CAP], w[:, kc, 1, q * 128:(q + 1) * 128],
                                                                        XgT[:, kc, :], start=(kc == 0), stop=(kc == 7)),
                         reads=[w_t, XgT_t[kc]], writes=[PS_t[pui]])
                k = ntmp % NTMP
                ntmp += 1
                g_, sg_, u_, tt = tg[k], tsg[k], tu[k], tmp_t[k]
                p.op("dve", lambda e, g_=g_, fc=fc, ex=ex, pgi=pgi: e.tensor_scalar(g_[:], PS[pgi][:, 0:CAP], bguT[:, fc, ex:ex + 1],
                                                                                  SW_LIMIT, ALU.add, ALU.min),
                     reads=[PS_t[pgi], bguT_t], writes=[tt])
                p.op("act", lambda e, g_=g_, sg_=sg_: e.activation(sg_[:], g_[:], ACTF.Sigmoid, scale=SW_ALPHA),
                     reads=[tt], writes=[tt])
                p.op("dve", lambda e, u_=u_, fc=fc, ex=ex, pui=pui: e.tensor_scalar(u_[:], PS[pui][:, 0:CAP],
                                                                                  bguT[:, 8 + fc, ex:ex + 1], SW_LIMIT,
                                                                                  ALU.add, ALU.min),
                     reads=[PS_t[pui], bguT_t], writes=[tt])
                p.op("dve", lambda e, u_=u_: e.tensor_scalar(u_[:], u_[:], -SW_LIMIT, 1.0, ALU.max, ALU.add),
                     reads=[tt], writes=[tt])
                p.op("dve", lambda e, g_=g_, sg_=sg_: e.tensor_tensor(g_[:], g_[:], sg_[:], ALU.mult),
                     reads=[tt], writes=[tt])
                p.op("dve", lambda e, g_=g_, u_=u_, fc=fc: e.tensor_tensor(actT[:, fc, :], g_[:], u_[:], ALU.mult),
                     reads=[tt], writes=[actT_t[fc]])
        for dh in range(2):
            w, w_t = wdt[nwd % NWD], wdt_t[nwd % NWD]
            nwd += 1
            wsrc, wsrc_t = wd_of(ex)
            src = wsrc.rearrange("(k p) n -> p k n", p=128)[:, :, dh * 512:(dh + 1) * 512]
            p.dma("pool", lambda e, w=w, src=src: e.dma_start(out=w[:], in_=src), reads=[wsrc_t], writes=[w_t])
            for jc in range(NJC):
                ps, ps_t = PS[ndn % 2], PS_t[ndn % 2]
                ndn += 1
                for kc in range(8):
                    p.op("pe", lambda e, ps=ps, w=w, kc=kc, jc=jc: e.matmul(ps[:], actT[:, kc, jc * 128:(jc + 1) * 128],
                                                                        w[:, kc, :], start=(kc == 0), stop=(kc == 7)),
                         reads=[actT_t[kc], w_t], writes=[ps_t])
                p.op("act", lambda e, ps=ps, jc=jc, dh=dh: e.activation(Yg[:, jc, dh * 512:(dh + 1) * 512], ps[:],
                                                                       ACTF.Copy, scale=gsl[:, jc:jc + 1]),
                     reads=[ps_t, gsl_t], writes=[Yg_t[jc]])
        for i in range(NCH):
            st, st_t = ST[nst % NST], ST_t[nst % NST]
            nst += 1
            psb = PS[7][:].bitcast(BF16)
            for jc in range(NJC):
                p.op("pe", lambda e, i=i, jc=jc, psb=psb, S=S: e.transpose(psb[:, jc * 128:(jc + 1) * 128],
                                                                     S[:, i, jc * 128:(jc + 1) * 128], C.ident_b[:]),
                     reads=[S_t[i], C.t], writes=[PS_t[7]])
            p.op("act", lambda e, st=st, psb=psb: e.copy(st[:], psb[:, 0:NJC * 128].rearrange("p (j t) -> p j t", j=NJC)),
                 reads=[PS_t[7]], writes=[st_t])
            for dh in range(2):
                ps, ps_t = PS[2 + nsc % 4], PS_t[2 + nsc % 4]
                nsc += 1
                for jc in range(NJC):
                    p.op("pe", lambda e, ps=ps, st=st, jc=jc, dh=dh: e.matmul(ps[:], st[:, jc, :],
                                                                          Yg[:, jc, dh * 512:(dh + 1) * 512],
                                                                          start=(jc == 0), stop=(jc == NJC - 1)),
                         reads=[st_t, Yg_t[jc]], writes=[ps_t])
                p.op("dve", lambda e, ps=ps, i=i, dh=dh: e.tensor_tensor(acc[:, i, dh * 512:(dh + 1) * 512],
                                                                       acc[:, i, dh * 512:(dh + 1) * 512], ps[:], ALU.add),
                     reads=[ps_t, acc_t[i]], writes=[acc_t[i]])

    p.end_phase()
    p.begin_phase()
    gt = p.sbuf("lng", [128, D], F32)
    bt = p.sbuf("lnb", [128, D], F32)
    gb_t = T()
    p.dma("sp", lambda e: e.dma_start(out=gt[:], in_=bcast_rows(lng_d)), writes=[gb_t])
    p.dma("sp", lambda e: e.dma_start(out=bt[:], in_=bcast_rows(lnb_d)), writes=[gb_t])
    lst = [p.sbuf("lst%d" % i, [128, 16], F32) for i in range(2)]
    lst_t = [T() for _ in range(2)]
    for i in range(NCH):
        emit_ln(p, acc[:, i, :], acc_t[i], gt, bt, gb_t, lst[i % 2], lst_t[i % 2], eng2="dve")
        p.dma("sp", lambda e, i=i: e.dma_start(out=h2_d[i * 128:(i + 1) * 128, :], in_=acc[:, i, :]),
              reads=[acc_t[i]], writes=[h2_dt])
    p.end_phase()
    p.end_phase()


def build_moe_only():
    nc = bass.Bass("TRN2", target_bir_lowering=False)
    h1 = nc.dram_tensor("h1", [NTOK, D], F32, kind="ExternalInput").ap()
    wr = nc.dram_tensor("wr", [D, NE], F32, kind="ExternalInput").ap()
    br = nc.dram_tensor("br", [NE], F32, kind="ExternalInput").ap()
    wgu = nc.dram_tensor("wgu", [NE, D, 2 * D], F32, kind="ExternalInput").ap()
    bgu = nc.dram_tensor("bgu", [NE, 2 * D], F32, kind="ExternalInput").ap()
    wd = nc.dram_tensor("wd", [NE, D, D], F32, kind="ExternalInput").ap()
    bd = nc.dram_tensor("bd", [NE, D], F32, kind="ExternalInput").ap()
    lng = nc.dram_tensor("lng", [D], F32, kind="ExternalInput").ap()
    lnb = nc.dram_tensor("lnb", [D], F32, kind="ExternalInput").ap()
    h2 = nc.dram_tensor("h2", [NTOK, D], F32, kind="ExternalOutput").ap()
    p = Prog(nc)
    C = Consts(p)
    wt_, wt2_ = T(), T()
    emit_moe(p, C, h1, T(), h2, T(), wr, br, lambda ex: (wgu[ex], wt_), bgu, lambda ex: (wd[ex], wt2_), bd, lng, lnb)
    p.finish()
    return nc


def emit_hT(p, C, PS, PS_t, h_d, h_dt, halo_d, halo_dt, hT, hT_t):
    p.begin_phase()
    xin = [p.sbuf("hx%d" % i, [128, D], F32) for i in range(2)]
    xin_t = [T() for _ in range(2)]
    hl = p.sbuf("hl", [32, D], F32); hl_t = T()
    p.dma("sp", lambda e: e.dma_start(out=hl[:], in_=halo_d), reads=[halo_dt], writes=[hl_t])
    for half in range(2):
        ps, ps_t = PS[half], PS_t[half]
        for q in range(4):
            dc = half * 4 + q
            p.op("pe", lambda e, dc=dc, q=q, ps=ps: e.transpose(ps[:, q * 32:(q + 1) * 32], hl[:, dc * 128:(dc + 1) * 128],
                                                              C.ident_f[0:32, 0:32]), reads=[hl_t, C.t], writes=[ps_t])
        p.op("dve", lambda e, ps=ps, half=half: e.tensor_copy(hT[:, half * 4:(half + 1) * 4, 0:32],
                                                             ps[:, 0:128].rearrange("p (q t) -> p q t", q=4)),
             reads=[ps_t], writes=[hT_t])
    for i in range(NCH):
        b = i % 2
        x, x_t = xin[b], xin_t[b]
        p.dma("sp", lambda e, x=x, i=i: e.dma_start(out=x[:], in_=h_d[i * 128:(i + 1) * 128, :]), reads=[h_dt], writes=[x_t])
        for half in range(2):
            ps, ps_t = PS[2 + half], PS_t[2 + half]
            for q in range(4):
                dc = half * 4 + q
                p.op("pe", lambda e, x=x, dc=dc, q=q, ps=ps: e.transpose(ps[:, q * 128:(q + 1) * 128],
                                                                     x[:, dc * 128:(dc + 1) * 128], C.ident_f[:]),
                     reads=[x_t, C.t], writes=[ps_t])
            if half == 0:
                p.op("dve", lambda e, ps=ps, i=i: e.tensor_copy(hT[:, 0:4, 32 + i * 128:32 + (i + 1) * 128],
                                                              ps[:].rearrange("p (q t) -> p q t", q=4)),
                     reads=[ps_t], writes=[hT_t])
            else:
                p.op("act", lambda e, ps=ps, i=i: e.copy(hT[:, 4:8, 32 + i * 128:32 + (i + 1) * 128],
                                                        ps[:].rearrange("p (q t) -> p q t", q=4)),
                     reads=[ps_t], writes=[hT_t])
    p.end_phase()


def load_rowsT(p, C, ps, ps_t, src_d, nrows, dst, dst_t, name):
    raw = p.sbuf(name, [nrows, D], F32); raw_t = T()
    p.dma("sp", lambda e: e.dma_start(out=raw[:], in_=src_d), writes=[raw_t])
    for c in range(8):
        p.op("pe", lambda e, c=c: e.transpose(ps[:, 0:nrows], raw[:, c * 128:(c + 1) * 128], C.ident_f[0:nrows, 0:nrows]),
             reads=[raw_t, C.t], writes=[ps_t])
        p.op("dve", lambda e, c=c: e.tensor_copy(dst[:, c, :], ps[:, 0:nrows]), reads=[ps_t], writes=[dst_t])


CONF_K = 31
TT = [(0, 32)] + [(32 + i * 512, 512) for i in range(4)]


def emit_conformer(p, C, h_d, h_dt, halo_d, halo_dt, hasprev_d, h1_d, h1_dt,
                   w1_d, w1_dt, b1_d, wdw_d, bdw_d, lng_d, lnb_d, w2_d, w2_dt, b2_d, mg_d, mb_d):
    p.begin_phase()
    PS = [p.psum("cps%d" % i, [128, 512], F32) for i in range(8)]
    PS_t = [T() for _ in range(8)]
    vT = p.sbuf("vT", [128, 8, NTOK], F32)
    vT_t = [T() for _ in range(8)]
    prm = p.sbuf("cprm", [128, 8, 8], F32); prm_t = T()
    wdwT = p.sbuf("wdwT", [128, 8, CONF_K], F32); wdwT_t = T()
    hp = p.sbuf("hasprev", [128, 1], F32)
    p.dma("sp", lambda e: e.dma_start(out=hp[:], in_=hasprev_d.partition_broadcast(128)), writes=[prm_t])
    p.begin_phase()
    praw = p.sbuf("praw", [8, D], F32); praw_t = T()
    srcs = [b1_d[0:D], b1_d[D:2 * D], bdw_d, lng_d, lnb_d]
    for j, s_ in enumerate(srcs):
        p.dma("sp", lambda e, j=j, s_=s_: e.dma_start(out=praw[j:j + 1, :], in_=s_.rearrange("(o n) -> o n", o=1)), writes=[praw_t])
    for c in range(8):
        p.op("pe", lambda e, c=c: e.transpose(PS[7][:, 0:5], praw[0:5, c * 128:(c + 1) * 128], C.ident_f[0:5, 0:5]),
             reads=[praw_t, C.t], writes=[PS_t[7]])
        p.op("dve", lambda e, c=c: e.tensor_copy(prm[:, c, 0:5], PS[7][:, 0:5]), reads=[PS_t[7]], writes=[prm_t])
    load_rowsT(p, C, PS[6], PS_t[6], wdw_d, CONF_K, wdwT, wdwT_t, "wdwraw")
    hT = p.sbuf("hT", [128, 8, 32 + NTOK], BF16); hT_t = T()
    emit_hT(p, C, PS, PS_t, h_d, h_dt, halo_d, halo_dt, hT, hT_t)
    w1t = [p.sbuf("w1t%d" % i, [128, 8, 2, 256], BF16) for i in range(2)]
    w1t_t = [T() for _ in range(2)]
    ut = [p.sbuf("ut%d" % i, [128, 32 + NTOK], F32) for i in range(2)]
    ut_t = [T() for _ in range(2)]
    sg = [p.sbuf("sg%d" % i, [128, 512], F32) for i in range(2)]
    sg_t = [T() for _ in range(2)]
    nsg = 0
    ptmp = p.sbuf("ptmp", [128, 512], F32); ptmp_t = T()
    for cp in range(4):
        w, w_t = w1t[cp % 2], w1t_t[cp % 2]
        for two in range(2):
            src = w1_d.rearrange("(k p) f -> p k f", p=128)[:, :, two * D + cp * 256:two * D + (cp + 1) * 256]
            p.dma("pool", lambda e, w=w, src=src, two=two: e.dma_start(out=w[:, :, two, :], in_=src), reads=[w1_dt], writes=[w_t])
        for q in range(2):
            cc = cp * 2 + q
            u, u_t = ut[cc % 2], ut_t[cc % 2]
            for ti, (c0, n) in enumerate(TT):
                pa, pa_t = PS[(2 * ti) % 6], PS_t[(2 * ti) % 6]
                pg, pg_t = PS[(2 * ti + 1) % 6], PS_t[(2 * ti + 1) % 6]
                for k in range(8):
                    p.op("pe", lambda e, pa=pa, w=w, k=k, q=q, c0=c0, n=n: e.matmul(
                        pa[:, 0:n], w[:, k, 0, q * 128:(q + 1) * 128], hT[:, k, c0:c0 + n], start=(k == 0), stop=(k == 7)),
                        reads=[w_t, hT_t], writes=[pa_t])
                for k in range(8):
                    p.op("pe", lambda e, pg=pg, w=w, k=k, q=q, c0=c0, n=n: e.matmul(
                        pg[:, 0:n], w[:, k, 1, q * 128:(q + 1) * 128], hT[:, k, c0:c0 + n], start=(k == 0), stop=(k == 7)),
                        reads=[w_t, hT_t], writes=[pg_t])
                s_, s_t = sg[nsg % 2], sg_t[nsg % 2]
                nsg += 1
                p.op("act", lambda e, s_=s_, pg=pg, n=n, cc=cc: e.activation(s_[:, 0:n], pg[:, 0:n], ACTF.Sigmoid,
                                                                           bias=prm[:, cc, 1:2]),
                     reads=[pg_t, prm_t], writes=[s_t])
                p.op("dve", lambda e, u=u, pa=pa, s_=s_, c0=c0, n=n, cc=cc: e.scalar_tensor_tensor(
                    u[:, c0:c0 + n], pa[:, 0:n], prm[:, cc, 0:1], s_[:, 0:n], ALU.add, ALU.mult),
                    reads=[pa_t, s_t, prm_t], writes=[u_t])
            p.op("dve", lambda e, u=u: e.tensor_scalar(u[:, 0:32], u[:, 0:32], hp[:, 0:1], None, ALU.mult),
                 reads=[u_t, prm_t], writes=[u_t])
            TS = 1536
            vt = T()
            for k in range(CONF_K):
                off = 32 - (CONF_K - 1) + k
                if k == 0:
                    p.op("dve", lambda e, u=u, cc=cc, off=off: e.tensor_scalar(
                        vT[:, cc, 0:TS], u[:, off:off + TS], wdwT[:, cc, 0:1], prm[:, cc, 2:3], ALU.mult, ALU.add),
                        reads=[u_t, wdwT_t, prm_t], writes=[vt])
                else:
                    p.op("dve", lambda e, u=u, cc=cc, off=off, k=k: e.scalar_tensor_tensor(
                        vT[:, cc, 0:TS], u[:, off:off + TS], wdwT[:, cc, k:k + 1], vT[:, cc, 0:TS], ALU.mult, ALU.add),
                        reads=[u_t, wdwT_t, vt], writes=[vt])
            vt2 = T()
            n2 = NTOK - TS
            for k in range(CONF_K):
                off = 32 + TS - (CONF_K - 1) + k
                if k == 0:
                    p.op("pool", lambda e, u=u, cc=cc, off=off: e.tensor_scalar(
                        vT[:, cc, TS:NTOK], u[:, off:off + n2], wdwT[:, cc, 0:1], prm[:, cc, 2:3], ALU.mult, ALU.add),
                        reads=[u_t, wdwT_t, prm_t], writes=[vt2])
                else:
                    p.op("pool", lambda e, u=u, cc=cc, off=off, k=k: e.tensor_scalar(
                        ptmp[:], u[:, off:off + n2], wdwT[:, cc, k:k + 1], None, ALU.mult),
                        reads=[u_t, wdwT_t], writes=[ptmp_t])
                    p.op("pool", lambda e, cc=cc: e.tensor_tensor(vT[:, cc, TS:NTOK], vT[:, cc, TS:NTOK], ptmp[:], ALU.add),
                         reads=[ptmp_t, vt2], writes=[vt2])
    p.end_phase()
    p.begin_phase()
    wT = p.sbuf("wT", [128, 8, NTOK], BF16)
    wT_t = [T() for _ in range(4)]
    ones_f = p.sbuf("ones_f", [128, 128], F32); ones_t = T()
    p.op("pool", lambda e: e.memset(ones_f[:], 1.0), writes=[ones_t])
    w2 = p.sbuf("w2", [128, 8, D], BF16); w2_t = T()
    for half in range(2):
        p.dma("pool", lambda e, half=half: e.dma_start(out=w2[:, :, half * 512:(half + 1) * 512],
                                                      in_=w2_d.rearrange("(k p) n -> p k n", p=128)[:, :, half * 512:(half + 1) * 512]),
              reads=[w2_dt], writes=[w2_t])
    b2B = p.sbuf("b2B", [128, D], F32)
    mgB = p.sbuf("mgB", [128, D], F32)
    mbB = p.sbuf("mbB", [128, D], F32)
    gb_t = T()
    p.dma("sp", lambda e: e.dma_start(out=b2B[:], in_=b2_d.partition_broadcast(128)), writes=[gb_t])
    p.dma("sp", lambda e: e.dma_start(out=mgB[:], in_=mg_d.partition_broadcast(128)), writes=[gb_t])
    p.dma("sp", lambda e: e.dma_start(out=mbB[:], in_=mb_d.partition_broadcast(128)), writes=[gb_t])
    sq = [p.sbuf("sq%d" % i, [128, 512], F32) for i in range(2)]
    sq_t = [T() for _ in range(2)]
    mu = p.sbuf("mu", [128, 512], F32)
    rstd = p.sbuf("rstd", [128, 512], F32)
    st_t = T()
    tmp = [p.sbuf("lt%d" % i, [128, 512], F32) for i in range(2)]
    tmp_t = [T() for _ in range(2)]
    nsq = 0
    for tt in range(4):
        c0 = tt * 512
        for cc in range(8):
            p.op("pe", lambda e, cc=cc, c0=c0: e.matmul(PS[0][:], ones_f[:], vT[:, cc, c0:c0 + 512], start=(cc == 0), stop=(cc == 7)),
                 reads=[ones_t, vT_t[cc]], writes=[PS_t[0]])
        for cc in range(8):
            s_, s_t = sq[nsq % 2], sq_t[nsq % 2]
            nsq += 1
            p.op("act", lambda e, s_=s_, cc=cc, c0=c0: e.activation(s_[:], vT[:, cc, c0:c0 + 512], ACTF.Square),
                 reads=[vT_t[cc]], writes=[s_t])
            p.op("pe", lambda e, s_=s_, cc=cc: e.matmul(PS[1][:], ones_f[:], s_[:], start=(cc == 0), stop=(cc == 7)),
                 reads=[ones_t, s_t], writes=[PS_t[1]])
        p.op("dve", lambda e: e.tensor_scalar(mu[:], PS[0][:], 1.0 / D, None, ALU.mult), reads=[PS_t[0]], writes=[st_t])
        p.op("dve", lambda e: e.tensor_tensor(rstd[:], mu[:], mu[:], ALU.mult), reads=[st_t], writes=[st_t])
        p.op("dve", lambda e: e.scalar_tensor_tensor(rstd[:], PS[1][:], 1.0 / D, rstd[:], ALU.mult, ALU.subtract),
             reads=[PS_t[1], st_t], writes=[st_t])
        p.op("act", lambda e: e.activation(rstd[:], rstd[:], ACTF.Sqrt, bias=LN_EPS), reads=[st_t], writes=[st_t])
        p.op("dve", lambda e: e.reciprocal(rstd[:], rstd[:]), reads=[st_t], writes=[st_t])
        for cc in range(8):
            t_, t_t = tmp[cc % 2], tmp_t[cc % 2]
            p.op("dve", lambda e, t_=t_, cc=cc, c0=c0: e.tensor_tensor(t_[:], vT[:, cc, c0:c0 + 512], mu[:], ALU.subtract),
                 reads=[vT_t[cc], st_t], writes=[t_t])
            p.op("dve", lambda e, t_=t_: e.tensor_tensor(t_[:], t_[:], rstd[:], ALU.mult), reads=[t_t, st_t], writes=[t_t])
            p.op("act", lambda e, t_=t_, cc=cc, c0=c0: e.activation(wT[:, cc, c0:c0 + 512], t_[:], ACTF.Silu,
                                                                  bias=prm[:, cc, 4:5], scale=prm[:, cc, 3:4]),
                 reads=[t_t, prm_t], writes=[wT_t[tt]])
    hx = [p.sbuf("chx%d" % i, [128, D], F32) for i in range(2)]
    hx_t = [T() for _ in range(2)]
    lst = [p.sbuf("clst%d" % i, [128, 16], F32) for i in range(2)]
    lst_t = [T() for _ in range(2)]
    for i in range(NCH):
        b = i % 2
        x, x_t = hx[b], hx_t[b]
        p.dma("sp", lambda e, x=x, i=i: e.dma_start(out=x[:], in_=h_d[i * 128:(i + 1) * 128, :]), reads=[h_dt], writes=[x_t])
        p.op("dve", lambda e, x=x: e.scalar_tensor_tensor(x[:], x[:], ALPHA, b2B[:], ALU.mult, ALU.add),
             reads=[x_t, gb_t], writes=[x_t])
        for dh in range(2):
            ps, ps_t = PS[2 + dh], PS_t[2 + dh]
            for cc in range(8):
                p.op("pe", lambda e, ps=ps, cc=cc, i=i, dh=dh: e.matmul(ps[:], wT[:, cc, i * 128:(i + 1) * 128],
                                                                    w2[:, cc, dh * 512:(dh + 1) * 512],
                                                                    start=(cc == 0), stop=(cc == 7)),
                     reads=[wT_t[i // 4], w2_t], writes=[ps_t])
            p.op("dve", lambda e, ps=ps, x=x, dh=dh: e.tensor_tensor(x[:, dh * 512:(dh + 1) * 512],
                                                                   x[:, dh * 512:(dh + 1) * 512], ps[:], ALU.add),
                 reads=[ps_t, x_t], writes=[x_t])
        emit_ln(p, x[:], x_t, mgB, mbB, gb_t, lst[b], lst_t[b], eng2="pool")
        p.dma("sp", lambda e, x=x, i=i: e.dma_start(out=h1_d[i * 128:(i + 1) * 128, :], in_=x[:]), reads=[x_t], writes=[h1_dt])
    p.end_phase()
    p.end_phase()


def build_conf_only():
    nc = bass.Bass("TRN2", target_bir_lowering=False)
    def inp(name, shape):
        return nc.dram_tensor(name, list(shape), F32, kind="ExternalInput").ap()
    h = inp("h", [NTOK, D]); halo = inp("halo", [32, D]); hasprev = inp("hasprev", [1])
    w1 = inp("w1", [D, 2 * D]); b1 = inp("b1", [2 * D]); wdw = inp("wdw", [CONF_K, D]); bdw = inp("bdw", [D])
    lng = inp("lng", [D]); lnb = inp("lnb", [D]); w2 = inp("w2", [D, D]); b2 = inp("b2", [D])
    mg = inp("mg", [D]); mb = inp("mb", [D])
    h1 = nc.dram_tensor("h1", [NTOK, D], F32, kind="ExternalOutput").ap()
    p = Prog(nc)
    C = Consts(p)
    emit_conformer(p, C, h, T(), halo, T(), hasprev, h1, T(), w1, T(), b1, wdw, bdw, lng, lnb, w2, T(), b2, mg, mb)
    p.finish()
    return nc


def bc_last(ap, n):
    return bass.AP(ap.tensor, ap.offset, [list(x) for x in ap.ap] + [[0, n]])


def bc_mid(ap, n):
    l = [list(x) for x in ap.ap]
    return bass.AP(ap.tensor, ap.offset, [l[0], [0, n]] + l[1:])


def emit_allgather(p, src_d, src_t, dst_d, dst_t):
    p.coll(lambda e: e.collective_compute("AllGather", ALU.bypass, replica_groups=[list(range(8))],
                                          ins=[src_d.opt()], outs=[dst_d.opt()]),
           reads=[src_t], writes=[dst_t])


def emit_halo(p, C, nc, h_d, h_dt, sel_d, halo_d, halo_dt, tag):
    src = nc.dram_tensor("halo_src_" + tag, [32, D], F32).ap()
    gat = nc.dram_tensor("halo_gat_" + tag, [256, D], F32).ap()
    src_t, gat_t = T(), T()
    p.dma("sp", lambda e: e.dma_start(out=src, in_=h_d[NTOK - 32:NTOK, :]), reads=[h_dt], writes=[src_t])
    emit_allgather(p, src, src_t, gat, gat_t)
    p.begin_phase()
    ps = [p.psum("hps%d" % i, [128, 512], F32) for i in range(2)]
    ps_t = [T(), T()]
    g = p.sbuf("hg", [128, 2, D], F32); g_t = T()
    sel = p.sbuf("hsel", [128, 2, 32], F32)
    ho = p.sbuf("hout", [32, D], F32); ho_t = T()
    p.dma("sp", lambda e: e.dma_start(out=g[:], in_=gat.rearrange("(k p) d -> p k d", p=128)), reads=[gat_t], writes=[g_t])
    p.dma("sp", lambda e: e.dma_start(out=sel[:], in_=sel_d.rearrange("(k p) j -> p k j", p=128)), writes=[g_t])
    for dh in range(2):
        for k in range(2):
            p.op("pe", lambda e, dh=dh, k=k: e.matmul(ps[dh][0:32, :], sel[:, k, :], g[:, k, dh * 512:(dh + 1) * 512],
                                                      start=(k == 0), stop=(k == 1)), reads=[g_t], writes=[ps_t[dh]])
        p.op("dve", lambda e, dh=dh: e.tensor_copy(ho[:, dh * 512:(dh + 1) * 512], ps[dh][0:32, :]), reads=[ps_t[dh]], writes=[ho_t])
    p.dma("sp", lambda e: e.dma_start(out=halo_d, in_=ho[:]), reads=[ho_t], writes=[halo_dt])
    p.end_phase()


NH = 16
HP = 64
COL_Z = 3 * D
COL_XBC = 4 * D
COL_DT = 4 * D + 1536


def emit_even(p, C, nc, tag, h_d, h_dt, halo_d, halo_dt, h1_d, h1_dt, win_d, win_dt, wout_d, wout_dt,
              scw_d, cw_d, cb_d, dtb_d, alog_d, dsk_d, nw_d, mg_d, mb_d, before_d, between_d):
    winv = win_d.rearrange("(k p) f -> p k f", p=128)
    woutv = wout_d.rearrange("(k p) f -> p k f", p=128)
    hacc_d = nc.dram_tensor("hacc_" + tag, [NTOK, D], F32).ap(); hacc_dt = T()
    st_src = nc.dram_tensor("stsrc_" + tag, [128, D + NH], F32).ap(); st_src_t = T()
    st_gat = nc.dram_tensor("stgat_" + tag, [8 * 128, D + NH], F32).ap(); st_gat_t = T()

    p.begin_phase()
    PS = [p.psum("xps%d" % i, [128, 512], F32) for i in range(8)]
    PS_t = [T() for _ in range(8)]
    hT = p.sbuf("xhT", [128, 8, 32 + NTOK], BF16); hT_t = T()
    emit_hT(p, C, PS, PS_t, h_d, h_dt, halo_d, halo_dt, hT, hT_t)
    p.begin_phase()
    scT = p.sbuf("scT", [128, 8, 3], F32); scT_t = T()
    load_rowsT(p, C, PS[7], PS_t[7], scw_d, 3, scT, scT_t, "scraw")
    yscT = p.sbuf("yscT", [128, 8, NTOK], BF16); yscT_t = [T() for _ in range(8)]
    wo1 = p.sbuf("wo1", [128, 8, D], BF16); wo1_t = T()
    for half in range(2):
        p.dma("pool", lambda e, half=half: e.dma_start(out=wo1[:, :, half * 512:(half + 1) * 512],
                                                      in_=woutv[:, 0:8, half * 512:(half + 1) * 512]), reads=[wout_dt], writes=[wo1_t])
    w3 = [p.sbuf("w3_%d" % i, [128, 8, 3, 256], BF16) for i in range(2)]
    w3_t = [T(), T()]
    pp = [p.sbuf("pp%d" % i, [128, 32 + NTOK], F32) for i in range(2)]
    pp_t = [T(), T()]
    po = [p.sbuf("po%d" % i, [128, 32 + NTOK], F32) for i in range(2)]
    po_t = [T(), T()]
    tp = [p.sbuf("tp%d" % i, [128, 512], F32) for i in range(2)]
    tp_t = [T(), T()]
    cc3 = [p.sbuf("cc3_%d" % i, [128, NTOK], F32) for i in range(2)]
    cc3_t = [T(), T()]
    ntp = 0
    nrot = 0
    for cp in range(4):
        w, w_t = w3[cp % 2], w3_t[cp % 2]
        for br in range(3):
            p.dma("pool", lambda e, w=w, br=br, cp=cp: e.dma_start(out=w[:, :, br, :], in_=winv[:, :, br * D + cp * 256:br * D + (cp + 1) * 256]),
                  reads=[win_dt], writes=[w_t])
        for q in range(2):
            cc = cp * 2 + q
            pp_, ppt = pp[cc % 2], pp_t[cc % 2]
            po_, pot = po[cc % 2], po_t[cc % 2]
            for ti, (c0, n) in enumerate(TT):
                banks = [(nrot + b) % 6 for b in range(3)]
                nrot += 3
                for br in range(3):
                    ps, ps_t = PS[banks[br]], PS_t[banks[br]]
                    for k in range(8):
                        p.op("pe", lambda e, ps=ps, w=w, k=k, q=q, br=br, c0=c0, n=n: e.matmul(
                            ps[:, 0:n], w[:, k, br, q * 128:(q + 1) * 128], hT[:, k, c0:c0 + n], start=(k == 0), stop=(k == 7)),
                            reads=[w_t, hT_t], writes=[ps_t])
                t_, t_t = tp[ntp % 2], tp_t[ntp % 2]
                ntp += 1
                p.op("act", lambda e, t_=t_, n=n, b1=banks[1]: e.copy(t_[:, 0:n], PS[b1][:, 0:n]), reads=[PS_t[banks[1]]], writes=[t_t])
                p.op("dve", lambda e, t_=t_, n=n, c0=c0, pp_=pp_, b0=banks[0]: e.tensor_tensor(pp_[:, c0:c0 + n], PS[b0][:, 0:n], t_[:, 0:n], ALU.mult),
                     reads=[PS_t[banks[0]], t_t], writes=[ppt])
                p.op("act", lambda e, n=n, c0=c0, po_=po_, b2=banks[2]: e.copy(po_[:, c0:c0 + n], PS[b2][:, 0:n]), reads=[PS_t[banks[2]]], writes=[pot])
            c_, c_t = cc3[cc % 2], cc3_t[cc % 2]
            for k in range(3):
                off = 32 - 2 + k
                if k == 0:
                    p.op("dve", lambda e, c_=c_, pp_=pp_, cc=cc, off=off: e.tensor_scalar(c_[:], pp_[:, off:off + NTOK], scT[:, cc, 0:1], None, ALU.mult),
                         reads=[ppt, scT_t], writes=[c_t])
                else:
                    p.op("dve", lambda e, c_=c_, pp_=pp_, cc=cc, off=off, k=k: e.scalar_tensor_tensor(
                        c_[:], pp_[:, off:off + NTOK], scT[:, cc, k:k + 1], c_[:], ALU.mult, ALU.add), reads=[ppt, scT_t, c_t], writes=[c_t])
            p.op("pool", lambda e, c_=c_, po_=po_, cc=cc: e.tensor_tensor(yscT[:, cc, :], c_[:], po_[:, 32:32 + NTOK], ALU.mult),
                 reads=[c_t, pot], writes=[yscT_t[cc]])
    hx = [p.sbuf("ehx%d" % i, [128, D], F32) for i in range(2)]
    hx_t = [T(), T()]
    for i in range(NCH):
        x, x_t = hx[i % 2], hx_t[i % 2]
        p.dma("sp", lambda e, x=x, i=i: e.dma_start(out=x[:], in_=h_d[i * 128:(i + 1) * 128, :]), reads=[h_dt], writes=[x_t])
        for dh in range(2):
            ps, ps_t = PS[6 + dh], PS_t[6 + dh]
            for cc in range(8):
                p.op("pe", lambda e, ps=ps, cc=cc, i=i, dh=dh: e.matmul(ps[:], yscT[:, cc, i * 128:(i + 1) * 128],
                                                                    wo1[:, cc, dh * 512:(dh + 1) * 512], start=(cc == 0), stop=(cc == 7)),
                     reads=[yscT_t[cc], wo1_t], writes=[ps_t])
            p.op("dve", lambda e, ps=ps, x=x, dh=dh: e.scalar_tensor_tensor(x[:, dh * 512:(dh + 1) * 512], x[:, dh * 512:(dh + 1) * 512],
                                                                          ALPHA, ps[:], ALU.mult, ALU.add), reads=[ps_t, x_t], writes=[x_t])
        p.dma("sp", lambda e, x=x, i=i: e.dma_start(out=hacc_d[i * 128:(i + 1) * 128, :], in_=x[:]), reads=[x_t], writes=[hacc_dt])
    p.end_phase()
    p.end_phase()

    p.begin_phase()
    PS = [p.psum("eps%d" % i, [128, 512], F32) for i in range(8)]
    PS_t = [T() for _ in range(8)]
    xs = p.sbuf("xs", [128, NCH, D], BF16); xs_t = [T() for _ in range(NCH)]
    btok = p.sbuf("btok", [128, NCH, 256], BF16); btok_t = [T() for _ in range(NCH)]
    BT = p.sbuf("BT", [128, 2, NTOK], BF16); BT_t = T()
    CT = p.sbuf("CT", [128, 2, NTOK], BF16); CT_t = T()
    dt = p.sbuf("dt", [128, NCH, NH], F32)
    da = p.sbuf("da", [128, NCH, NH], F32)
    Ea = p.sbuf("Ea", [128, NCH, NH], F32)
    dec = p.sbuf("dec", [128, NCH, NH], F32)
    wst = p.sbuf("wst", [128, NCH, NH], F32)
    dd_t = T()
    state = p.sbuf("state", [128, D], F32); state_t = T()
    state_b = p.sbuf("state_b", [128, D], BF16); state_bt = T()
    logD = p.sbuf("logD", [128, NH], F32); logD_t = T()
    U_f = p.sbuf("U_f", [128, 128], F32)
    Ls_f = p.sbuf("Ls_f", [128, 128], F32)
    ones_f = p.sbuf("eones_f", [128, 128], F32)
    cst_t = T()
    p.op("pool", lambda e: e.memset(ones_f[:], 1.0), writes=[cst_t])
    p.op("pool", lambda e: e.memset(U_f[:], 1.0), writes=[cst_t])
    p.op("pool", lambda e: e.affine_select(U_f[:], U_f[:], [[1, 128]], ALU.is_ge, 0.0, base=0, channel_multiplier=-1),
         reads=[cst_t], writes=[cst_t])
    p.op("pool", lambda e: e.memset(Ls_f[:], 1.0), writes=[cst_t])
    p.op("pool", lambda e: e.affine_select(Ls_f[:], Ls_f[:], [[-1, 128]], ALU.is_gt, 0.0, base=0, channel_multiplier=1),
         reads=[cst_t], writes=[cst_t])
    p.op("pool", lambda e: e.memset(state[:], 0.0), writes=[state_t])
    hp16 = p.sbuf("hp16", [128, 4, NH], F32); hp16_t = T()
    p.dma("sp", lambda e: e.dma_start(out=hp16[:, 0, :], in_=dtb_d.partition_broadcast(128)), writes=[hp16_t])
    p.dma("sp", lambda e: e.dma_start(out=hp16[:, 1, :], in_=alog_d.partition_broadcast(128)), writes=[hp16_t])
    p.dma("sp", lambda e: e.dma_start(out=hp16[:, 2, :], in_=dsk_d.partition_broadcast(128)), writes=[hp16_t])
    p.op("act", lambda e: e.activation(hp16[:, 1, :], hp16[:, 1, :], ACTF.Exp), reads=[hp16_t], writes=[hp16_t])
    p.op("dve", lambda e: e.tensor_scalar(hp16[:, 1, :], hp16[:, 1, :], -1.0, None, ALU.mult), reads=[hp16_t], writes=[hp16_t])
    xdd = [p.sbuf("xdd%d" % i, [128, D], BF16) for i in range(2)]
    xdd_t = [T(), T()]
    sz = p.sbuf("sz", [128, NCH, D], BF16); sz_t = [T() for _ in range(NCH)]

    def state_step(i):
        x_, x_t = xdd[i % 2], xdd_t[i % 2]
        p.op("dve", lambda e: e.tensor_tensor(x_[:].rearrange("p (h q) -> p h q", h=NH), xs[:, i, :].rearrange("p (h q) -> p h q", h=NH),
                                              bc_last(wst[:, i, :], HP), ALU.mult), reads=[xs_t[i], dd_t], writes=[x_t])
        for g in range(2):
            p.op("pe", lambda e, g=g: e.matmul(PS[5 + g][:], btok[:, i, g * 128:(g + 1) * 128], x_[:, g * 512:(g + 1) * 512],
                                               start=True, stop=True), reads=[btok_t[i], x_t], writes=[PS_t[5 + g]])
        p.op("dve", lambda e: e.tensor_tensor(state[:].rearrange("p (h q) -> p h q", h=NH), state[:].rearrange("p (h q) -> p h q", h=NH),
                                              bc_last(dec[:, i, :], HP), ALU.mult), reads=[state_t, dd_t], writes=[state_t])
        for g in range(2):
            p.op("dve", lambda e, g=g: e.tensor_tensor(state[:, g * 512:(g + 1) * 512], state[:, g * 512:(g + 1) * 512],
                                                       PS[5 + g][:], ALU.add), reads=[state_t, PS_t[5 + g]], writes=[state_t])
        p.op("act", lambda e: e.copy(state_b[:], state[:]), reads=[state_t], writes=[state_bt])

    p.begin_phase()
    hT = p.sbuf("ehT", [128, 8, 32 + NTOK], BF16); hT_t = T()
    emit_hT(p, C, PS, PS_t, h_d, h_dt, halo_d, halo_dt, hT, hT_t)
    p.begin_phase()
    wz = p.sbuf("wz", [128, 8, D], BF16); wz_t = T()
    for half in range(2):
        p.dma("pool", lambda e, half=half: e.dma_start(out=wz[:, :, half * 512:(half + 1) * 512],
                                                      in_=winv[:, :, COL_Z + half * 512:COL_Z + (half + 1) * 512]), reads=[win_dt], writes=[wz_t])
    for i in range(NCH):
        for dh in range(2):
            ps, ps_t = PS[(i * 2 + dh) % 4], PS_t[(i * 2 + dh) % 4]
            for k in range(8):
                p.op("pe", lambda e, ps=ps, k=k, i=i, dh=dh: e.matmul(ps[:], hT[:, k, 32 + i * 128:32 + (i + 1) * 128],
                                                                  wz[:, k, dh * 512:(dh + 1) * 512], start=(k == 0), stop=(k == 7)),
                     reads=[hT_t, wz_t], writes=[ps_t])
            p.op("act", lambda e, ps=ps, i=i, dh=dh: e.activation(sz[:, i, dh * 512:(dh + 1) * 512], ps[:], ACTF.Silu),
                 reads=[ps_t], writes=[sz_t[i]])
    p.end_phase()
    p.begin_phase()
    craw = p.sbuf("craw", [5, 1536], F32); craw_t = T()
    p.dma("sp", lambda e: e.dma_start(out=craw[0:4, :], in_=cw_d), writes=[craw_t])
    p.dma("sp", lambda e: e.dma_start(out=craw[4:5, :], in_=cb_d.rearrange("(o n) -> o n", o=1)), writes=[craw_t])
    cwT = p.sbuf("cwT", [128, 12, 8], F32); cwT_t = T()
    for mc in range(12):
        p.op("pe", lambda e, mc=mc: e.transpose(PS[7][:, 0:5], craw[0:5, mc * 128:(mc + 1) * 128], C.ident_f[0:5, 0:5]),
             reads=[craw_t, C.t], writes=[PS_t[7]])
        p.op("dve", lambda e, mc=mc: e.tensor_copy(cwT[:, mc, 0:5], PS[7][:, 0:5]), reads=[PS_t[7]], writes=[cwT_t])
    wt = [p.sbuf("ewt%d" % i, [128, 8, 256], BF16) for i in range(2)]
    wt_t = [T(), T()]
    xc = [p.sbuf("xc%d" % i, [128, 32 + NTOK], F32) for i in range(2)]
    xc_t = [T(), T()]
    cv = [p.sbuf("cv%d" % i, [128, NTOK], F32) for i in range(2)]
    cv_t = [T(), T()]
    xo = [p.sbuf("xo%d" % i, [128, NTOK], BF16) for i in range(2)]
    xo_t = [T(), T()]
    for mp in range(6):
        w, w_t = wt[mp % 2], wt_t[mp % 2]
        p.dma("pool", lambda e, w=w, mp=mp: e.dma_start(out=w[:], in_=winv[:, :, COL_XBC + mp * 256:COL_XBC + (mp + 1) * 256]),
              reads=[win_dt], writes=[w_t])
        for q in range(2):
            mc = mp * 2 + q
            x_, x_t = xc[mc % 2], xc_t[mc % 2]
            for ti, (c0, n) in enumerate(TT):
                ps, ps_t = PS[ti % 4], PS_t[ti % 4]
                for k in range(8):
                    p.op("pe", lambda e, ps=ps, w=w, k=k, q=q, c0=c0, n=n: e.matmul(
                        ps[:, 0:n], w[:, k, q * 128:(q + 1) * 128], hT[:, k, c0:c0 + n], start=(k == 0), stop=(k == 7)),
                        reads=[w_t, hT_t], writes=[ps_t])
                p.op("act", lambda e, x_=x_, ps=ps, c0=c0, n=n: e.copy(x_[:, c0:c0 + n], ps[:, 0:n]), reads=[ps_t], writes=[x_t])
            c_, c_t = cv[mc % 2], cv_t[mc % 2]
            for k in range(4):
                off = 32 - 3 + k
                if k == 0:
                    p.op("dve", lambda e, c_=c_, x_=x_, mc=mc, off=off: e.tensor_scalar(
                        c_[:], x_[:, off:off + NTOK], cwT[:, mc, 0:1], cwT[:, mc, 4:5], ALU.mult, ALU.add),
                        reads=[x_t, cwT_t], writes=[c_t])
                else:
                    p.op("dve", lambda e, c_=c_, x_=x_, mc=mc, off=off, k=k: e.scalar_tensor_tensor(
                        c_[:], x_[:, off:off + NTOK], cwT[:, mc, k:k + 1], c_[:], ALU.mult, ALU.add),
                        reads=[x_t, cwT_t, c_t], writes=[c_t])
            if mc >= 10:
                g = mc - 10
                p.op("act", lambda e, c_=c_, g=g: e.activation(CT[:, g, :], c_[:], ACTF.Silu), reads=[c_t], writes=[CT_t])
                continue
            if mc < 8:
                o_, o_t = xo[mc % 2], xo_t[mc % 2]
                p.op("act", lambda e, o_=o_, c_=c_: e.activation(o_[:], c_[:], ACTF.Silu), reads=[c_t], writes=[o_t])
                src_ap, src_t = o_[:], o_t
            else:
                g = mc - 8
                p.op("act", lambda e, c_=c_, g=g: e.activation(BT[:, g, :], c_[:], ACTF.Silu), reads=[c_t], writes=[BT_t])
                src_ap, src_t = BT[:, g, :], BT_t
            for ih in range(2):
                psb = PS[4 + ih][:].bitcast(BF16)
                for j in range(8):
                    i = ih * 8 + j
                    p.op("pe", lambda e, psb=psb, j=j, i=i, src_ap=src_ap: e.transpose(
                        psb[:, j * 128:(j + 1) * 128], src_ap[:, i * 128:(i + 1) * 128], C.ident_b[:]),
                        reads=[src_t, C.t], writes=[PS_t[4 + ih]])
                if mc < 8:
                    dst = xs[:, ih * 8:(ih + 1) * 8, mc * 128:(mc + 1) * 128]
                    dst_ts = xs_t[ih * 8:(ih + 1) * 8]
                else:
                    dst = btok[:, ih * 8:(ih + 1) * 8, (mc - 8) * 128:(mc - 7) * 128]
                    dst_ts = btok_t[ih * 8:(ih + 1) * 8]
                if ih == 0:
                    p.op("dve", lambda e, psb=psb, dst=dst: e.tensor_copy(dst, psb.rearrange("p (j c) -> p j c", j=8)),
                         reads=[PS_t[4 + ih]], writes=dst_ts)
                else:
                    p.op("act", lambda e, psb=psb, dst=dst: e.copy(dst, psb.rearrange("p (j c) -> p j c", j=8)),
                         reads=[PS_t[4 + ih]], writes=dst_ts)
    wdt = p.sbuf("wdt", [128, 8, NH], BF16); wdt_t = T()
    p.dma("pool", lambda e: e.dma_start(out=wdt[:], in_=winv[:, :, COL_DT:COL_DT + NH]), reads=[win_dt], writes=[wdt_t])
    for i in range(NCH):
        for k in range(8):
            p.op("pe", lambda e, i=i, k=k: e.matmul(PS[6][:, i * NH:(i + 1) * NH], hT[:, k, 32 + i * 128:32 + (i + 1) * 128],
                                                    wdt[:, k, :], start=(k == 0), stop=(k == 7)),
                 reads=[hT_t, wdt_t], writes=[PS_t[6]])
    sp_ = p.sbuf("sp_", [128, NCH, NH], F32)
    cum = p.sbuf("cum", [128, NCH, NH], F32)
    tot = p.sbuf("tot", [128, NCH, NH], F32)
    p.op("dve", lambda e: e.tensor_tensor(dt[:], PS[6][:, 0:NCH * NH].rearrange("p (i h) -> p i h", i=NCH),
                                          bc_mid(hp16[:, 0, :], NCH), ALU.add), reads=[PS_t[6], hp16_t], writes=[dd_t])
    p.op("act", lambda e: e.activation(sp_[:], dt[:], ACTF.Abs), reads=[dd_t], writes=[dd_t])
    p.op("act", lambda e: e.activation(sp_[:], sp_[:], ACTF.Exp, scale=-1.0), reads=[dd_t], writes=[dd_t])
    p.op("act", lambda e: e.activation(sp_[:], sp_[:], ACTF.Ln, bias=1.0), reads=[dd_t], writes=[dd_t])
    p.op("dve", lambda e: e.scalar_tensor_tensor(dt[:], dt[:], 0.0, sp_[:], ALU.max, ALU.add), reads=[dd_t], writes=[dd_t])
    p.op("dve", lambda e: e.tensor_tensor(da[:], dt[:], bc_mid(hp16[:, 1, :], NCH), ALU.mult), reads=[dd_t, hp16_t], writes=[dd_t])
    for i in range(NCH):
        p.op("pe", lambda e, i=i: e.matmul(PS[7][:, i * NH:(i + 1) * NH], U_f[:], da[:, i, :], start=True, stop=True),
             reads=[dd_t, cst_t], writes=[PS_t[7]])
        p.op("pe", lambda e, i=i: e.matmul(PS[7][:, 256 + i * NH:256 + (i + 1) * NH], ones_f[:], da[:, i, :], start=True, stop=True),
             reads=[dd_t, cst_t], writes=[PS_t[7]])
    p.op("dve", lambda e: e.tensor_copy(cum[:], PS[7][:, 0:256].rearrange("p (i h) -> p i h", i=NCH)), reads=[PS_t[7]], writes=[dd_t])
    p.op("dve", lambda e: e.tensor_copy(tot[:], PS[7][:, 256:512].rearrange("p (i h) -> p i h", i=NCH)), reads=[PS_t[7]], writes=[dd_t])
    p.op("dve", lambda e: e.reduce_sum(logD[:], tot[:].rearrange("p i h -> p h i"), AX.X), reads=[dd_t], writes=[logD_t])
    p.op("act", lambda e: e.activation(Ea[:], cum[:], ACTF.Exp), reads=[dd_t], writes=[dd_t])
    p.op("act", lambda e: e.activation(dec[:], tot[:], ACTF.Exp), reads=[dd_t], writes=[dd_t])
    p.op("dve", lambda e: e.tensor_tensor(wst[:], tot[:], cum[:], ALU.subtract), reads=[dd_t], writes=[dd_t])
    p.op("act", lambda e: e.activation(wst[:], wst[:], ACTF.Exp), reads=[dd_t], writes=[dd_t])
    p.op("dve", lambda e: e.tensor_tensor(wst[:], wst[:], dt[:], ALU.mult), reads=[dd_t], writes=[dd_t])
    for i in range(NCH):
        state_step(i)
    p.dma("sp", lambda e: e.dma_start(out=st_src[:, 0:D], in_=state[:]), reads=[state_t], writes=[st_src_t])
    p.dma("sp", lambda e: e.dma_start(out=st_src[:, D:D + NH], in_=logD[:]), reads=[logD_t], writes=[st_src_t])
    p.end_phase()
    p.end_phase()
    emit_allgather(p, st_src, st_src_t, st_gat, st_gat_t)


    p.begin_phase()
    lall = p.sbuf("lall", [128, 8, NH], F32); lall_t = T()
    p.dma("sp", lambda e: e.dma_start(out=lall[:], in_=st_gat.rearrange("(r p) c -> p r c", p=128)[:, :, D:D + NH]),
          reads=[st_gat_t], writes=[lall_t])
    bef = p.sbuf("bef", [128, 8], F32)
    btw = p.sbuf("btw", [128, 64], F32)
    p.dma("sp", lambda e: e.dma_start(out=bef[:], in_=before_d.partition_broadcast(128)), writes=[lall_t])
    p.dma("sp", lambda e: e.dma_start(out=btw[:], in_=between_d.partition_broadcast(128)), writes=[lall_t])
    coef = p.sbuf("coef", [128, 8, NH], F32); coef_t = T()
    p.op("pool", lambda e: e.memset(coef[:], 0.0), writes=[coef_t])
    for r in range(8):
        for r2 in range(8):
            p.op("dve", lambda e, r=r, r2=r2: e.scalar_tensor_tensor(coef[:, r, :], lall[:, r2, :], btw[:, r * 8 + r2:r * 8 + r2 + 1],
                                                                   coef[:, r, :], ALU.mult, ALU.add), reads=[lall_t, coef_t], writes=[coef_t])
    p.op("act", lambda e: e.activation(coef[:], coef[:], ACTF.Exp), reads=[coef_t], writes=[coef_t])
    p.op("dve", lambda e: e.tensor_tensor(coef[:], coef[:], bc_last(bef[:, :], NH), ALU.mult), reads=[coef_t, lall_t], writes=[coef_t])
    p.op("pool", lambda e: e.memset(state[:], 0.0), reads=[state_t], writes=[state_t])
    sr = [p.sbuf("sr%d" % i, [128, D], F32) for i in range(2)]
    sr_t = [T(), T()]
    for r in range(8):
        s_, s_t = sr[r % 2], sr_t[r % 2]
        p.dma("sp", lambda e, s_=s_, r=r: e.dma_start(out=s_[:], in_=st_gat[r * 128:(r + 1) * 128, 0:D]), reads=[st_gat_t], writes=[s_t])
        p.op("dve", lambda e, s_=s_, r=r: e.tensor_tensor(s_[:].rearrange("p (h q) -> p h q", h=NH), s_[:].rearrange("p (h q) -> p h q", h=NH),
                                                        bc_last(coef[:, r, :], HP), ALU.mult), reads=[s_t, coef_t], writes=[s_t])
        p.op("dve", lambda e, s_=s_: e.tensor_tensor(state[:], state[:], s_[:], ALU.add), reads=[s_t, state_t], writes=[state_t])
    p.op("act", lambda e: e.copy(state_b[:], state[:]), reads=[state_t], writes=[state_bt])
    p.end_phase()

    p.begin_phase()
    wo2 = p.sbuf("wo2", [128, 8, D], BF16); wo2_t = T()
    for half in range(2):
        p.dma("pool", lambda e, half=half: e.dma_start(out=wo2[:, :, half * 512:(half + 1) * 512],
                                                      in_=woutv[:, 8:16, half * 512:(half + 1) * 512]), reads=[wout_dt], writes=[wo2_t])
    DB = p.sbuf("DB", [128, D], F32)
    nwB = p.sbuf("nwB", [128, D], F32)
    mgB = p.sbuf("emgB", [128, D], F32)
    mbB = p.sbuf("embB", [128, D], F32)
    gb_t = T()
    p.op("pool", lambda e: e.memset(DB[:], 1.0), writes=[gb_t])
    p.op("dve", lambda e: e.tensor_tensor(DB[:].rearrange("p (h q) -> p h q", h=NH), DB[:].rearrange("p (h q) -> p h q", h=NH),
                                          bc_last(hp16[:, 2, :], HP), ALU.mult), reads=[gb_t, hp16_t], writes=[gb_t])
    p.dma("sp", lambda e: e.dma_start(out=nwB[:], in_=nw_d.partition_broadcast(128)), writes=[gb_t])
    p.dma("sp", lambda e: e.dma_start(out=mgB[:], in_=mg_d.partition_broadcast(128)), writes=[gb_t])
    p.dma("sp", lambda e: e.dma_start(out=mbB[:], in_=mb_d.partition_broadcast(128)), writes=[gb_t])
    xd = [p.sbuf("xd%d" % i, [128, D], BF16) for i in range(2)]; xd_t = [T(), T()]
    cbm = [p.sbuf("cbm%d" % i, [128, 2, 128], F32) for i in range(2)]; cbm_t = [T(), T()]
    lh = [p.sbuf("lh%d" % i, [128, 128], F32) for i in range(4)]; lh_t = [T() for _ in range(4)]
    Eb = [p.sbuf("Eb%d" % i, [128, 4, 128], F32) for i in range(2)]; Eb_t = [T(), T()]
    MT = [p.sbuf("MT%d" % i, [128, 4, 128], BF16) for i in range(2)]; MT_t = [T(), T()]
    yb = [p.sbuf("yb%d" % i, [128, D], F32) for i in range(2)]; yb_t = [T(), T()]
    t2 = [p.sbuf("t2_%d" % i, [128, D], F32) for i in range(2)]; t2_t = [T(), T()]
    ybf = [p.sbuf("ybf%d" % i, [128, D], BF16) for i in range(2)]; ybf_t = [T(), T()]
    yT = [p.sbuf("yT%d" % i, [128, 8, 128], BF16) for i in range(2)]; yT_t = [T(), T()]
    rr = [p.sbuf("rr%d" % i, [128, 4], F32) for i in range(2)]; rr_t = [T(), T()]
    hx = [p.sbuf("e2hx%d" % i, [128, D], F32) for i in range(2)]; hx_t = [T(), T()]
    lst = [p.sbuf("elst%d" % i, [128, 16], F32) for i in range(2)]; lst_t = [T(), T()]
    nlh = 0
    neb = 0
    for i in range(NCH):
        b = i % 2
        xd_, xdt = xd[b], xd_t[b]
        p.op("dve", lambda e, xd_=xd_, i=i: e.tensor_tensor(xd_[:].rearrange("p (h q) -> p h q", h=NH), xs[:, i, :].rearrange("p (h q) -> p h q", h=NH),
                                                          bc_last(dt[:, i, :], HP), ALU.mult), reads=[xs_t[i], dd_t], writes=[xdt])
        cb_, cbt = cbm[b], cbm_t[b]
        y_, yt = yb[b], yb_t[b]
        for g in range(2):
            p.op("pe", lambda e, g=g, i=i: e.matmul(PS[0][:, g * 128:(g + 1) * 128], BT[:, g, i * 128:(i + 1) * 128],
                                                    CT[:, g, i * 128:(i + 1) * 128], start=True, stop=True),
                 reads=[BT_t, CT_t], writes=[PS_t[0]])
            p.op("dve", lambda e, g=g, cb_=cb_: e.tensor_tensor(cb_[:, g, :], PS[0][:, g * 128:(g + 1) * 128], U_f[:], ALU.mult),
                 reads=[PS_t[0], cst_t], writes=[cbt])
            for hq in range(2):
                for h4 in range(4):
                    h = g * 8 + hq * 4 + h4
                    l_, l_t = lh[nlh % 4], lh_t[nlh % 4]
                    nlh += 1
                    p.op("dve", lambda e, l_=l_, i=i, h=h: e.tensor_scalar(l_[:], Ls_f[:], da[:, i, h:h + 1], None, ALU.mult),
                         reads=[cst_t, dd_t], writes=[l_t])
                    p.op("pe", lambda e, l_=l_, hq=hq, h4=h4: e.matmul(PS[1 + hq][:, h4 * 128:(h4 + 1) * 128], l_[:], U_f[:], start=True, stop=True),
                         reads=[l_t, cst_t], writes=[PS_t[1 + hq]])
                E_, E_t = Eb[neb % 2], Eb_t[neb % 2]
                M_, M_t = MT[neb % 2], MT_t[neb % 2]
                neb += 1
                p.op("act", lambda e, E_=E_, hq=hq: e.activation(E_[:], PS[1 + hq][:].rearrange("p (a s) -> p a s", a=4), ACTF.Exp),
                     reads=[PS_t[1 + hq]], writes=[E_t])
                p.op("pool", lambda e, E_=E_, M_=M_, cb_=cb_, g=g: e.tensor_tensor(M_[:], E_[:], bc_mid(cb_[:, g, :], 4), ALU.mult),
                     reads=[E_t, cbt], writes=[M_t])
                for h4 in range(4):
                    h = g * 8 + hq * 4 + h4
                    hh = hq * 4 + h4
                    p.op("pe", lambda e, M_=M_, h4=h4, h=h, hh=hh, g=g, xd_=xd_: e.matmul(PS[3 + g][:, hh * HP:(hh + 1) * HP], M_[:, h4, :],
                                                                                      xd_[:, h * HP:(h + 1) * HP], start=True, stop=True),
                         reads=[M_t, xdt], writes=[PS_t[3 + g]])
            p.op("pe", lambda e, g=g, i=i: e.matmul(PS[5 + g][:], CT[:, g, i * 128:(i + 1) * 128], state_b[:, g * 512:(g + 1) * 512],
                                                    start=True, stop=True), reads=[CT_t, state_bt], writes=[PS_t[5 + g]])
            p.op("dve", lambda e, g=g, i=i, y_=y_: e.tensor_tensor(y_[:, g * 512:(g + 1) * 512].rearrange("p (h q) -> p h q", h=8),
                                                                 PS[5 + g][:].rearrange("p (h q) -> p h q", h=8),
                                                                 bc_last(Ea[:, i, g * 8:(g + 1) * 8], HP), ALU.mult),
                 reads=[PS_t[5 + g], dd_t], writes=[yt])
            p.op("dve", lambda e, g=g, y_=y_: e.tensor_tensor(y_[:, g * 512:(g + 1) * 512], y_[:, g * 512:(g + 1) * 512], PS[3 + g][:], ALU.add),
                 reads=[PS_t[3 + g], yt], writes=[yt])
        t_, tt_ = t2[b], t2_t[b]
        p.op("pool", lambda e, t_=t_, i=i: e.tensor_tensor(t_[:], xs[:, i, :], DB[:], ALU.mult), reads=[xs_t[i], gb_t], writes=[tt_])
        p.op("pool", lambda e, t_=t_, y_=y_: e.tensor_tensor(y_[:], y_[:], t_[:], ALU.add), reads=[tt_, yt], writes=[yt])
        p.op("pool", lambda e, y_=y_, i=i: e.tensor_tensor(y_[:], y_[:], sz[:, i, :], ALU.mult), reads=[yt, sz_t[i]], writes=[yt])
        r_, r_t = rr[b], rr_t[b]
        p.op("act", lambda e, t_=t_, y_=y_: e.activation(t_[:], y_[:], ACTF.Square), reads=[yt, tt_], writes=[tt_])
        p.op("dve", lambda e, r_=r_, t_=t_: e.reduce_sum(r_[:, 0:2], t_[:].rearrange("p (g c) -> p g c", g=2), AX.X), reads=[tt_], writes=[r_t])
        p.op("act", lambda e, r_=r_: e.activation(r_[:, 0:2], r_[:, 0:2], ACTF.Sqrt, bias=LN_EPS, scale=1.0 / 512), reads=[r_t], writes=[r_t])
        p.op("dve", lambda e, r_=r_: e.reciprocal(r_[:, 0:2], r_[:, 0:2]), reads=[r_t], writes=[r_t])
        yf, yft = ybf[b], ybf_t[b]
        for g in range(2):
            p.op("dve", lambda e, g=g, yf=yf, y_=y_, r_=r_: e.scalar_tensor_tensor(yf[:, g * 512:(g + 1) * 512], y_[:, g * 512:(g + 1) * 512],
                                                                                 r_[:, g:g + 1], nwB[:, g * 512:(g + 1) * 512], ALU.mult, ALU.mult),
                 reads=[yt, r_t, gb_t], writes=[yft])
        psb = PS[7][:].bitcast(BF16)
        for k in range(8):
            p.op("pe", lambda e, k=k, yf=yf, psb=psb: e.transpose(psb[:, k * 128:(k + 1) * 128], yf[:, k * 128:(k + 1) * 128], C.ident_b[:]),
                 reads=[yft, C.t], writes=[PS_t[7]])
        yT_, yTt = yT[b], yT_t[b]
        p.op("act", lambda e, yT_=yT_, psb=psb: e.copy(yT_[:], psb.rearrange("p (k t) -> p k t", k=8)), reads=[PS_t[7]], writes=[yTt])
        x, x_t = hx[b], hx_t[b]
        p.dma("sp", lambda e, x=x, i=i: e.dma_start(out=x[:], in_=hacc_d[i * 128:(i + 1) * 128, :]), reads=[hacc_dt], writes=[x_t])
        for dh in range(2):
            for k in range(8):
                p.op("pe", lambda e, k=k, dh=dh, yT_=yT_: e.matmul(PS[1 + dh][:], yT_[:, k, :], wo2[:, k, dh * 512:(dh + 1) * 512],
                                                                 start=(k == 0), stop=(k == 7)), reads=[yTt, wo2_t], writes=[PS_t[1 + dh]])
            p.op("dve", lambda e, x=x, dh=dh: e.tensor_tensor(x[:, dh * 512:(dh + 1) * 512], x[:, dh * 512:(dh + 1) * 512], PS[1 + dh][:], ALU.add),
                 reads=[PS_t[1 + dh], x_t], writes=[x_t])
        emit_ln(p, x[:], x_t, mgB, mbB, gb_t, lst[b], lst_t[b], eng2="pool")
        p.dma("sp", lambda e, x=x, i=i: e.dma_start(out=h1_d[i * 128:(i + 1) * 128, :], in_=x[:]), reads=[x_t], writes=[h1_dt])
        state_step(i)
    p.end_phase()
    p.end_phase()


def core_consts(c):
    b, pos = c // 4, c % 4
    sel = np.zeros((256, 32), np.float32)
    if pos != 0:
        for j in range(32):
            sel[(c - 1) * 32 + j, j] = 1.0
    before = np.zeros((8,), np.float32)
    between = np.zeros((8, 8), np.float32)
    for r in range(8):
        if r // 4 == b and r < c:
            before[r] = 1.0
            for r2 in range(8):
                if r2 // 4 == b and r < r2 < c:
                    between[r, r2] = 1.0
    hasprev = np.array([1.0 if pos != 0 else 0.0], np.float32)
    return dict(sel=sel, before=before, between=between.reshape(64), hasprev=hasprev)


def build_even_only():
    nc = bass.Bass("TRN2", target_bir_lowering=False)
    def inp(name, shape):
        return nc.dram_tensor(name, list(shape), F32, kind="ExternalInput").ap()
    h = inp("h", [NTOK, D]); sel = inp("sel", [256, 32]); before = inp("before", [8]); between = inp("between", [64])
    win = inp("win", [D, 5648]); wout = inp("wout", [2 * D, D])
    scw = inp("scw", [3, D]); cw = inp("cw", [4, 1536]); cb = inp("cb", [1536])
    dtb = inp("dtb", [NH]); alog = inp("alog", [NH]); dsk = inp("dsk", [NH]); nw = inp("nw", [D])
    mg = inp("mg", [D]); mb = inp("mb", [D])
    h1 = nc.dram_tensor("h1", [NTOK, D], F32, kind="ExternalOutput").ap()
    halo = nc.dram_tensor("halo_i", [32, D], F32).ap()
    p = Prog(nc)
    C = Consts(p)
    halo_t = T()
    emit_halo(p, C, nc, h, T(), sel, halo, halo_t, "t0")
    emit_even(p, C, nc, "t0", h, T(), halo, halo_t, h1, T(), win, T(), wout, T(), scw, cw, cb, dtb, alog, dsk, nw, mg, mb, before, between)
    p.finish()
    return nc


SMALL = [("sc_conv_w", [2, 3, D]), ("ssm_conv_w", [2, 4, 1536]), ("ssm_conv_b", [2, 1536]), ("ssm_dt_bias", [2, NH]),
         ("ssm_a_log", [2, NH]), ("ssm_d", [2, NH]), ("ssm_norm_w", [2, D]), ("conf_b_pw1", [2, 2 * D]),
         ("conf_w_dw", [2, CONF_K, D]), ("conf_b_dw", [2, D]), ("conf_ln_g", [2, D]), ("conf_ln_b", [2, D]),
         ("conf_b_pw2", [2, D]), ("router_w", [DEPTH, D, NE]), ("router_b", [DEPTH, NE]), ("exp_b_gu", [DEPTH, NE, 2 * D]),
         ("exp_b_down", [DEPTH, NE, D]), ("ln_mix_g", [DEPTH, D]), ("ln_mix_b", [DEPTH, D]), ("ln_ffn_g", [DEPTH, D]),
         ("ln_ffn_b", [DEPTH, D])]


def build_full(nl=DEPTH):
    nc = bass.Bass("TRN2", target_bir_lowering=False)

    def inp(name, shape):
        return nc.dram_tensor(name, list(shape), F32, kind="ExternalInput").ap()

    def internal(name, shape):
        return nc.dram_tensor(name, list(shape), F32).ap()

    x = inp("x", [NTOK, D])
    halo0 = inp("halo0", [32, D])
    sel = inp("sel", [256, 32]); before = inp("before", [8]); between = inp("between", [64]); hasprev = inp("hasprev", [1])
    win = inp("mix_w_in", [2, D, 5648]); wout = inp("mix_w_out", [2, 2 * D, D])
    pw1 = inp("conf_w_pw1", [2, D, 2 * D]); pw2 = inp("conf_w_pw2", [2, D, D])
    wgu = inp("exp_w_gu", [DEPTH, NE, D, 2 * D]); wd = inp("exp_w_down", [DEPTH, NE, D, D])
    sm = {name: inp(name, shape) for name, shape in SMALL}
    out = nc.dram_tensor("out", [NTOK, D], F32, kind="ExternalOutput").ap()

    p = Prog(nc)
    C = Consts(p)
    wt_ = T()
    h_cur, h_cur_t = x, T()
    for l in range(nl):
        j = l // 2
        tag = "L%d" % l
        if l == 0:
            halo, halo_t = halo0, T()
        else:
            halo = internal("halo_" + tag, [32, D]); halo_t = T()
            emit_halo(p, C, nc, h_cur, h_cur_t, sel, halo, halo_t, tag)
        hmid = internal("hmid_" + tag, [NTOK, D]); hmid_t = T()
        if l % 2 == 0:
            emit_even(p, C, nc, tag, h_cur, h_cur_t, halo, halo_t, hmid, hmid_t, win[j], wt_, wout[j], wt_,
                      sm["sc_conv_w"][j], sm["ssm_conv_w"][j], sm["ssm_conv_b"][j], sm["ssm_dt_bias"][j], sm["ssm_a_log"][j],
                      sm["ssm_d"][j], sm["ssm_norm_w"][j], sm["ln_mix_g"][l], sm["ln_mix_b"][l], before, between)
        else:
            emit_conformer(p, C, h_cur, h_cur_t, halo, halo_t, hasprev, hmid, hmid_t, pw1[j], wt_, sm["conf_b_pw1"][j],
                           sm["conf_w_dw"][j], sm["conf_b_dw"][j], sm["conf_ln_g"][j], sm["conf_ln_b"][j], pw2[j], wt_,
                           sm["conf_b_pw2"][j], sm["ln_mix_g"][l], sm["ln_mix_b"][l])
        if l == nl - 1:
            h_next, h_next_t = out, T()
        else:
            h_next, h_next_t = internal("h_" + tag, [NTOK, D]), T()
        emit_moe(p, C, hmid, hmid_t, h_next, h_next_t, sm["router_w"][l], sm["router_b"][l],
                 lambda ex, l=l: (wgu[l, ex], wt_), sm["exp_b_gu"][l], lambda ex, l=l: (wd[l, ex], wt_), sm["exp_b_down"][l],
                 sm["ln_ffn_g"][l], sm["ln_ffn_b"][l])
        h_cur, h_cur_t = h_next, h_next_t
    p.finish()
    return nc


_NC_CACHE = {}
BIG = ["mix_w_in", "mix_w_out", "conf_w_pw1", "conf_w_pw2", "exp_w_gu", "exp_w_down"]


def kernel(_nl=DEPTH, **inputs):
    f = np.float32
    A = lambda k: np.ascontiguousarray(np.asarray(inputs[k], dtype=f))
    x = A("x").reshape(8, NTOK, D)
    shared = {name: A(name) for name, _ in SMALL}
    for k in BIG:
        shared[k] = A(k)
    zeros = np.zeros((32, D), f)
    in_maps = []
    for c in range(8):
        cc = core_consts(c)
        halo0 = x[c - 1, NTOK - 32:, :] if c % 4 != 0 else zeros
        m = dict(x=x[c], halo0=np.ascontiguousarray(halo0), sel=cc["sel"], before=cc["before"], between=cc["between"],
                 hasprev=cc["hasprev"])
        m.update(shared)
        in_maps.append(m)
    if _nl not in _NC_CACHE:
        _NC_CACHE[_nl] = build_full(_nl)
    res = run_bass_kernel_spmd(_NC_CACHE[_nl], in_maps, core_ids=list(range(8)))
    outs = [np.asarray(res.results[c]["out"], dtype=f) for c in range(8)]
    return np.stack(outs).reshape(2, 4 * NTOK, D)
```
